# Optimizing a Trainium2 kernel written in Bass

```python
import math
import jax, jax.numpy as jnp
from jax import lax
import numpy as np

D_MODEL = 1024
BATCH = 4
SEQ = 8192
DEPTH = 1

D_FOURIER = D_MODEL // 2
FOURIER_GROUPS = 4
D_SSM = D_MODEL // 2
SSM_GROUP = 16
SSM_GROUPS = D_SSM // SSM_GROUP
SSM_STATE = 64
N_DIRECTIONS = 2
DT_MIN = 1e-3
DT_MAX = 1e-1
N_BRANCHES = 2
D_IN = D_FOURIER + D_SSM + N_BRANCHES * D_MODEL
N_EXPERTS = 16
D_EXPERT = 2816
EC_CAPACITY = 2
PLE_DIM = 256
RMS_EPS = 1e-6

kernel_name = 'hybrid_fourier_s5_expert_choice_block'


def rmsnorm(x, g):
    xf = x.astype(jnp.float32)
    y = xf * lax.rsqrt(jnp.mean(xf * xf, axis=-1, keepdims=True) + RMS_EPS)
    return (y * g.astype(jnp.float32)).astype(x.dtype)


def fourier_mix(u):
    b, s, _ = u.shape
    ug = u.astype(jnp.float32).reshape(b, s, FOURIER_GROUPS, D_FOURIER // FOURIER_GROUPS)
    ug = jnp.transpose(ug, (0, 2, 1, 3))
    f = jnp.fft.fft2(ug, axes=(-2, -1), norm='ortho').real
    f = jnp.transpose(f, (0, 2, 1, 3)).reshape(b, s, D_FOURIER)
    return f.astype(u.dtype)


def _linear_recurrence(c1, c2):
    a1, b1 = c1
    a2, b2 = c2
    return a1 * a2, a2 * b1 + b2


def ssm_direction(ug, a_re, a_im, log_dt, b_re, b_im, c_re, c_im, reverse):
    f32 = jnp.float32
    lam = lax.complex(a_re.astype(f32), a_im.astype(f32))
    dt = jnp.exp(log_dt.astype(f32))[:, None]
    lam_bar = jnp.exp(lam * dt)
    b_bar = ((lam_bar - 1.0) / lam)[..., None] * lax.complex(b_re.astype(f32), b_im.astype(f32))
    bu = jnp.einsum('bsgi,gni->bsgn', ug.astype(jnp.complex64), b_bar)
    a = jnp.broadcast_to(lam_bar, (1, ug.shape[1]) + lam_bar.shape)
    _, states = lax.associative_scan(_linear_recurrence, (a, bu), reverse=reverse, axis=1)
    c = lax.complex(c_re.astype(f32), c_im.astype(f32))
    return jnp.einsum('bsgn,gon->bsgo', states, c).real


def ssm_branch(u, a_re, a_im, log_dt, b_re, b_im, c_re, c_im, d, w_glu):
    bsz, s, _ = u.shape
    uf = u.astype(jnp.float32)
    ug = uf.reshape(bsz, s, SSM_GROUPS, SSM_GROUP)
    y = d.astype(jnp.float32) * uf
    for direction, rev in enumerate((False, True)):
        y = y + ssm_direction(ug, a_re[direction], a_im[direction], log_dt[direction],
                              b_re[direction], b_im[direction], c_re[direction], c_im[direction],
                              rev).reshape(bsz, s, D_SSM)
    y = jax.nn.gelu(y).astype(u.dtype)
    val, gate = jnp.split(y @ w_glu, 2, axis=-1)
    return val * jax.nn.sigmoid(gate)


def expert_choice_ffn(h, w_router, w_gate, w_up, w_down):
    bsz, s, d = h.shape
    cap = EC_CAPACITY * s // N_EXPERTS
    affinity = jax.nn.softmax((h @ w_router).astype(jnp.float32), axis=-1)
    vals, idx = lax.top_k(jnp.swapaxes(affinity, 1, 2), cap)
    bidx = jnp.arange(bsz)[:, None, None]
    xg = h[bidx, idx]
    hid = jax.nn.silu(jnp.einsum('becd,edf->becf', xg, w_gate)) * jnp.einsum('becd,edf->becf', xg, w_up)
    y = jnp.einsum('becf,efd->becd', hid, w_down) * vals[..., None].astype(h.dtype)
    flat = (bidx * s + idx).reshape(-1)
    out = jnp.zeros((bsz * s, d), h.dtype).at[flat].add(y.reshape(-1, d))
    return out.reshape(bsz, s, d)


def setup_inputs(seed: int = 0) -> dict:
    key = jax.random.key(seed)
    ks = jax.random.split(key, 24)

    def nrm(k, shape, scale):
        return jax.random.normal(k, shape, jnp.float32) * scale

    L = DEPTH
    G, N = SSM_GROUPS, SSM_STATE
    return {
        'x': nrm(ks[0], (BATCH, SEQ, D_MODEL), 1.0),
        'p': nrm(ks[1], (DEPTH, BATCH, SEQ, PLE_DIM), 1.0),
        'g_mix': 1.0 + nrm(ks[2], (L, D_MODEL), 0.01),
        'w_in': nrm(ks[3], (L, D_MODEL, D_IN), D_MODEL ** -0.5),
        'w_fourier': nrm(ks[4], (L, D_FOURIER, D_MODEL), D_FOURIER ** -0.5),
        'ssm_a_re': -0.5 + nrm(ks[5], (L, N_DIRECTIONS, G, N), 0.01),
        'ssm_a_im': math.pi * jnp.arange(N, dtype=jnp.float32) + nrm(ks[6], (L, N_DIRECTIONS, G, N), 0.01),
        'ssm_log_dt': jax.random.uniform(ks[7], (L, N_DIRECTIONS, G), jnp.float32,
                                         math.log(DT_MIN), math.log(DT_MAX)),
        'ssm_b_re': nrm(ks[8], (L, N_DIRECTIONS, G, N, SSM_GROUP), (2 * SSM_GROUP) ** -0.5),
        'ssm_b_im': nrm(ks[9], (L, N_DIRECTIONS, G, N, SSM_GROUP), (2 * SSM_GROUP) ** -0.5),
        'ssm_c_re': nrm(ks[10], (L, N_DIRECTIONS, G, SSM_GROUP, N), SSM_STATE ** -0.5),
        'ssm_c_im': nrm(ks[11], (L, N_DIRECTIONS, G, SSM_GROUP, N), SSM_STATE ** -0.5),
        'ssm_d': nrm(ks[12], (L, D_SSM), 1.0),
        'w_glu': nrm(ks[13], (L, D_SSM, 2 * D_MODEL), D_SSM ** -0.5),
        'w_out': nrm(ks[14], (L, D_MODEL, D_MODEL), D_MODEL ** -0.5),
        'g_ffn': 1.0 + nrm(ks[15], (L, D_MODEL), 0.01),
        'w_router': nrm(ks[16], (L, D_MODEL, N_EXPERTS), D_MODEL ** -0.5),
        'w_exp_gate': nrm(ks[17], (L, N_EXPERTS, D_MODEL, D_EXPERT), D_MODEL ** -0.5),
        'w_exp_up': nrm(ks[18], (L, N_EXPERTS, D_MODEL, D_EXPERT), D_MODEL ** -0.5),
        'w_exp_down': nrm(ks[19], (L, N_EXPERTS, D_EXPERT, D_MODEL), D_EXPERT ** -0.5),
        'g_ple': 1.0 + nrm(ks[20], (L, D_MODEL), 0.01),
        'w_ple_gate': nrm(ks[21], (L, D_MODEL, D_MODEL), D_MODEL ** -0.5),
        'w_ple_proj': nrm(ks[22], (L, PLE_DIM, D_MODEL), PLE_DIM ** -0.5),
        'g_final': 1.0 + nrm(ks[23], (D_MODEL,), 0.01),
    }


def reference(x, p, g_mix, w_in, w_fourier, ssm_a_re, ssm_a_im, ssm_log_dt, ssm_b_re, ssm_b_im,
              ssm_c_re, ssm_c_im, ssm_d, w_glu, w_out, g_ffn, w_router, w_exp_gate, w_exp_up,
              w_exp_down, g_ple, w_ple_gate, w_ple_proj, g_final):
    for i in range(DEPTH):
        h = rmsnorm(x, g_mix[i])
        z = h @ w_in[i]
        u_f, u_s, gates = jnp.split(z, [D_FOURIER, D_FOURIER + D_SSM], axis=-1)
        y_f = fourier_mix(u_f) @ w_fourier[i]
        y_s = ssm_branch(u_s, ssm_a_re[i], ssm_a_im[i], ssm_log_dt[i], ssm_b_re[i], ssm_b_im[i],
                         ssm_c_re[i], ssm_c_im[i], ssm_d[i], w_glu[i])
        g_f, g_s = jnp.split(jax.nn.sigmoid(gates), N_BRANCHES, axis=-1)
        x = x + (g_f * y_f + g_s * y_s) @ w_out[i]
        x = x + expert_choice_ffn(rmsnorm(x, g_ffn[i]), w_router[i], w_exp_gate[i],
                                  w_exp_up[i], w_exp_down[i])
        e = p[i] @ w_ple_proj[i]
        x = x + jax.nn.sigmoid(rmsnorm(x, g_ple[i]) @ w_ple_gate[i]) * e
    return rmsnorm(x, g_final)
```

```python
import math
import contextlib
import numpy as np
import ml_dtypes
import concourse.bass as bass
import concourse.mybir as mybir
from concourse.bass_utils import run_bass_kernel_spmd

F32 = mybir.dt.float32
BF16 = mybir.dt.bfloat16
I32 = mybir.dt.int32
ALU = mybir.AluOpType
ACTF = mybir.ActivationFunctionType

S = 8192
D = 1024
NT = 64
NE = 16
CAP = 1024
DEXP = 2816
NFC = DEXP // 128
TB = 512
NBLK = S // TB
BIG = 1.0e6
TWO_PI = 2 * math.pi
CW1 = 6.28125
CW2 = float(TWO_PI - 6.28125)
NDS = 48
ESPLIT = 4


class MK:
    def __init__(self, nc):
        self.nc = nc
        self.ops = []
        self.es = contextlib.ExitStack()

    def sb(self, name, shape, dt=F32):
        return self.es.enter_context(self.nc.sbuf_tensor(name, list(shape), dt))

    def ps(self, name, shape, dt=F32):
        return self.es.enter_context(self.nc.psum_tensor(name, list(shape), dt))

    def dram(self, name, shape, dt, kind="Internal"):
        return self.nc.dram_tensor(name, list(shape), dt, kind=kind).ap()

    def op(self, eng, emit, R=(), W=()):
        self.ops.append([eng, emit, tuple(R), tuple(W), False])

    def dma(self, eng, emit, R=(), W=()):
        self.ops.append([eng, emit, tuple(R), tuple(W), True])

    def barrier(self):
        self.ops.append(None)

    def tt(self, eng, out, a, b, op, R, W):
        self.op(eng, lambda e: e.tensor_tensor(out, a, b, op), R, W)

    def ts(self, eng, out, a, s1, s2, op0, op1, R, W, accum=None):
        if op1 is None:
            self.op(eng, lambda e: e.tensor_scalar(out, a, s1, None, op0), R, W)
        elif accum is None:
            self.op(eng, lambda e: e.tensor_scalar(out, a, s1, s2, op0, op1), R, W)
        else:
            self.op(eng, lambda e: e.tensor_scalar(out, a, s1, s2, op0, op1, accum_out=accum), R, W)

    def stt(self, eng, out, a, s, b, op0, op1, R, W):
        self.op(eng, lambda e: e.scalar_tensor_tensor(out, a, s, b, op0, op1), R, W)

    def cp(self, eng, out, a, R, W):
        if eng == "act":
            self.op(eng, lambda e: e.copy(out, a), R, W)
        else:
            self.op(eng, lambda e: e.tensor_copy(out, a), R, W)

    def act(self, out, a, func, R, W, bias=None, scale=None, accum=None):
        kw = {}
        if bias is not None:
            kw["bias"] = bias
        if scale is not None:
            kw["scale"] = scale
        if accum is not None:
            kw["accum_out"] = accum
        self.op("act", lambda e: e.activation(out, a, func, **kw), R, W)

    def memset(self, eng, out, val, W):
        self.op(eng, lambda e: e.memset(out, val), (), W)

    def mm(self, out, pairs, R, W):
        n = len(pairs)
        for i, (l, r) in enumerate(pairs):
            self.op("pe", (lambda l=l, r=r, i=i: (lambda e: e.matmul(out, l, r, start=(i == 0), stop=(i == n - 1))))(), R, W)

    def tr(self, out, a, ident, R, W):
        self.op("pe", lambda e: e.transpose(out, a, ident), R, W)

    def ld(self, eng, out, a, R, W, slow=False):
        if slow:
            self.dma(eng, lambda e: e.dma_start(out=out, in_=a, allow_slow_non_contiguous=True), R, W)
        else:
            self.dma(eng, lambda e: e.dma_start(out=out, in_=a), R, W)

    def finalize(self, final_wait_keys=()):
        nc = self.nc
        engs = ["pe", "act", "dve", "pool", "sp"]
        sems = {e: self.es.enter_context(nc.semaphore("sem_" + e)) for e in engs}
        dsems = [self.es.enter_context(nc.semaphore("dsem%d" % i)) for i in range(NDS)]
        last_w = {}
        readers = {}
        cnt = {e: 0 for e in engs}
        dlast = {}
        ndma = 0
        plan = {e: [] for e in engs}
        pend = {e: [] for e in engs}
        ev = []
        for i, o in enumerate(self.ops):
            if o is None:
                ev.append(None)
                bw = [("c", e, cnt[e]) for e in engs if cnt[e] > 0] + [("d", k, v) for k, v in dlast.items()]
                for e in engs:
                    pend[e] = list(bw)
                continue
            eng, emit, R, W, is_dma = o
            deps = set()
            for r in R:
                if r in last_w:
                    deps.add(last_w[r])
            for w in W:
                if w in last_w:
                    deps.add(last_w[w])
                for rd in readers.get(w, ()):
                    deps.add(rd)
            deps.discard(i)
            waits = [ev[d] for d in deps]
            if is_dma:
                k = ndma % NDS
                val = 16 * (ndma // NDS + 1)
                ndma += 1
                e_ = ("d", k, val)
                dlast[k] = val
                if val > 16:
                    waits.append(("d", k, val - 16))
            else:
                cnt[eng] += 1
                e_ = ("c", eng, cnt[eng])
            ev.append(e_)
            if pend[eng]:
                waits = waits + pend[eng]
                pend[eng] = []
            plan[eng].append((emit, waits, e_))
            for r in R:
                readers.setdefault(r, []).append(i)
            for w in W:
                last_w[w] = i
                readers[w] = []
        final_waits = [ev[last_w[k]] for k in final_wait_keys if k in last_w]

        def run_engine(engname, engobj):
            known = {}

            def do_wait(w):
                kind, a, v = w
                key = (kind, a)
                if known.get(key, 0) >= v:
                    return
                if kind == "c":
                    if a == engname and engname == "pe":
                        return
                    engobj.wait_ge(sems[a], v)
                else:
                    engobj.wait_ge(dsems[a], v)
                known[key] = v

            for emit, waits, e_ in plan[engname]:
                for w in waits:
                    do_wait(w)
                ins = emit(engobj)
                if e_[0] == "c":
                    ins.then_inc(sems[e_[1]], 1)
                else:
                    ins.then_inc(dsems[e_[1]], 16)
            if engname == "sp":
                for w in final_waits:
                    do_wait(w)

        with nc.Block() as block:
            @block.tensor
            def _(e):
                run_engine("pe", e)

            @block.scalar
            def _(e):
                run_engine("act", e)

            @block.vector
            def _(e):
                run_engine("dve", e)

            @block.gpsimd
            def _(e):
                run_engine("pool", e)

            @block.sync
            def _(e):
                run_engine("sp", e)
        self.es.close()
        return {e: len(plan[e]) for e in engs}


class Carve:
    def __init__(self, arena, width, tag):
        self.a = arena
        self.o = 0
        self.w = width
        self.tag = tag
        self.n = 0

    def _take(self, nf):
        assert self.o + nf <= self.w, (self.tag, self.o, nf, self.w)
        v = self.a[:, self.o:self.o + nf]
        self.o += nf
        self.n += 1
        return v

    @staticmethod
    def _shape(v, shape):
        if len(shape) == 1:
            return v
        names = "abcd"[:len(shape)]
        kw = {names[i]: shape[i] for i in range(1, len(shape))}
        return v.rearrange("p (%s) -> p %s" % (" ".join(names), " ".join(names)), **kw)

    def f32(self, *shape):
        n = int(np.prod(shape))
        return self._shape(self._take(n), shape)

    def bf16(self, *shape):
        n = int(np.prod(shape))
        v = self._take((n + 1) // 2).bitcast(BF16)[:, 0:n]
        return self._shape(v, shape)

    def i32(self, *shape):
        n = int(np.prod(shape))
        return self._shape(self._take(n).bitcast(I32), shape)


def host_consts():
    c = {}
    c["ident_bf"] = np.eye(128, dtype=np.float32).astype(ml_dtypes.bfloat16)
    c["ident_f"] = np.eye(128, dtype=np.float32)
    a = np.arange(64)[:, None, None, None, None]
    off = np.arange(2)[None, :, None, None, None]
    n1 = np.arange(64)[None, None, :, None, None]
    off2 = np.arange(2)[None, None, None, :, None]
    k1 = np.arange(64)[None, None, None, None, :]
    tok = 128 * n1 + 2 * a + off
    ang = (2 * np.pi / S) * ((k1 * tok) % S).astype(np.float64)
    delta = (off == off2).astype(np.float64)
    tre = (np.cos(ang) * delta).reshape(64, 128, 128)
    tim = (-np.sin(ang) * delta).reshape(64, 128, 128)
    c["t1"] = np.stack([tre, tim], axis=2).astype(np.float32).astype(ml_dtypes.bfloat16)
    n2 = np.arange(128)[:, None]
    k2 = np.arange(128)[None, :]
    a3 = (2 * np.pi / 128) * ((n2 * k2) % 128)
    cs, sn = np.cos(a3) / 1024.0, np.sin(a3) / 1024.0
    r3 = np.stack([np.concatenate([cs, -sn], axis=1), np.concatenate([sn, cs], axis=1)], axis=1)
    c["r3"] = r3.astype(np.float32).astype(ml_dtypes.bfloat16)
    c["cs128"] = np.stack([np.cos(a3), np.sin(a3)], axis=1).astype(np.float32)
    c["iota"] = np.broadcast_to(np.arange(TB + 1, dtype=np.float32)[None, :], (128, TB + 1)).copy()
    p = np.arange(128)
    same = (p[:, None] // 8) == (p[None, :] // 8)
    c["blk8"] = same.astype(np.float32)
    c["tri8"] = (same & (p[:, None] < p[None, :])).astype(np.float32)
    c["erow"] = ((p // 8) * CAP - 1 - BIG).astype(np.float32).reshape(128, 1)
    return c


CONST_SPECS = [("ident_bf", [128, 128], BF16), ("ident_f", [128, 128], F32), ("t1", [64, 128, 2, 128], BF16),
               ("r3", [128, 2, 256], BF16), ("cs128", [128, 2, 128], F32), ("iota", [128, TB + 1], F32),
               ("blk8", [128, 128], F32), ("tri8", [128, 128], F32), ("erow", [128, 1], F32)]

W_SPECS = [("g_mix", [1, D]), ("w_in", [1, D, 3072]), ("w_fourier", [1, 512, D]), ("ssm_a_re", [1, 2, 32, 64]),
           ("ssm_a_im", [1, 2, 32, 64]), ("ssm_log_dt", [1, 2, 32]), ("ssm_b_re", [1, 2, 32, 64, 16]),
           ("ssm_b_im", [1, 2, 32, 64, 16]), ("ssm_c_re", [1, 2, 32, 16, 64]), ("ssm_c_im", [1, 2, 32, 16, 64]),
           ("ssm_d", [1, 512]), ("w_glu", [1, 512, 2048]), ("w_out", [1, D, D]), ("g_ffn", [1, D]),
           ("w_router", [1, D, NE]), ("w_exp_gate", [1, NE, D, DEXP]), ("w_exp_up", [1, NE, D, DEXP]),
           ("w_exp_down", [1, NE, DEXP, D]), ("g_ple", [1, D]), ("w_ple_gate", [1, D, D]),
           ("w_ple_proj", [1, 256, D]), ("g_final", [D])]


def build(stage=99, dbg=False):
    nc = bass.Bass("TRN2", target_bir_lowering=False)
    m = MK(nc)
    x = m.dram("x", [S, D], F32, "ExternalInput")
    pin = m.dram("p", [S, 256], F32, "ExternalInput")
    class _LazyW(dict):
        def __missing__(self, n):
            self[n] = m.dram(n, dict(W_SPECS)[n], F32, "ExternalInput")
            return self[n]
    w = _LazyW()
    cst = {n: m.dram(n, shp, dt, "ExternalInput") for n, shp, dt in CONST_SPECS}
    out = m.dram("out", [S, D], F32, "ExternalOutput")
    dbgo = {}

    def dbg_out(name, shape, dt=F32):
        dbgo[name] = m.dram(name, shape, dt, "ExternalOutput")
        return dbgo[name]

    _regs = {}

    def _bc(e):
        if 'bc' not in _regs:
            _regs['bc'] = e.to_reg(NE * CAP - 1)
        return _regs['bc']

    BS = m.dram("BS", [64, 2, 128, 512], BF16)
    YS = m.dram("YS", [S, 512], BF16)
    X1 = m.dram("X1", [S, D], F32)
    H2 = m.dram("H2", [S, D], BF16)
    AFs = m.dram("AFs", [NE, S], F32)
    XG = m.dram("XG", [NE * CAP, D], BF16)
    YE = m.dram("YE", [NE * CAP, D], BF16)

    xC = x.rearrange("(p j) d -> j p d", j=64)
    pC = pin.rearrange("(p j) d -> j p d", j=64)
    outC = out.rearrange("(p j) d -> j p d", j=64)
    X1C = X1.rearrange("(p j) d -> j p d", j=64)
    H2C = H2.rearrange("(p j) d -> j p d", j=64)
    YSC = YS.rearrange("(p j) d -> j p d", j=64)
    xA = x.rearrange("(n1 n2) d -> n2 n1 d", n2=128)

    PW = 7450
    pers = m.sb("pers", [128, PW], F32)
    pc = Carve(pers, PW, "pers")
    ident_bf = pc.bf16(128)
    ident_f = pc.f32(128)
    gbc = {n: pc.f32(D) for n in ("g_mix", "g_ffn", "g_ple", "g_final")}
    AFF_TM = pc.f32(NT, NE)
    VALM = pc.f32(NT, NE)
    POSI = pc.i32(NT, NE)
    eps_t = pc.f32(1)
    ones_t = pc.f32(1)
    AW = 212800 // 4 - PW - 10
    arena = m.sb("arena", [128, AW], F32)
    PB = [m.ps("pb%d" % i, [128, 512], F32) for i in range(8)]

    def pbf(i, *shape):
        v = PB[i][:, :].bitcast(BF16)
        n = int(np.prod(shape))
        return Carve._shape(v[:, 0:n], shape)

    m.ld("sp", ident_bf, cst["ident_bf"], [], ["ident_bf"])
    m.ld("sp", ident_f, cst["ident_f"], [], ["ident_f"])
    for n in gbc:
        src = w[n][0:1, :] if n != "g_final" else w[n].rearrange("(o d) -> o d", o=1)
        m.ld("sp", gbc[n], src.partition_broadcast(128), [], ["gbc_" + n])
    m.memset("dve", eps_t, 1e-6, ["eps"])
    m.memset("dve", ones_t, 1.0, ["ones"])

    def load_cast(dst3, src3, key, nsplit):
        A = dst3.shape[1]
        step = max(1, A // nsplit)
        for a0 in range(0, A, step):
            a1 = min(A, a0 + step)
            m.dma("pool", (lambda d=dst3[:, a0:a1, :], s=src3[:, a0:a1, :]: (lambda e: e.dma_start(out=d, in_=s)))(), [], [key])

    def rmsnorm(xt, kx, g, kg, outt, kout, junk, kjunk, ss, kss, eng2="dve"):
        m.act(junk, xt, ACTF.Square, [kx], [kjunk, kss], accum=ss)
        m.ts("dve", ss, ss, 1.0 / D, eps_t[:, 0:1], ALU.mult, ALU.add, [kss, "eps"], [kss])
        m.act(ss, ss, ACTF.Sqrt, [kss], [kss])
        m.op("dve", lambda e: e.reciprocal(ss, ss), [kss], [kss])
        m.stt(eng2 if eng2 == "dve" else "dve", outt, xt, ss[:, 0:1], g, ALU.mult, ALU.mult, [kx, kss, kg], [kout])

    ca = Carve(arena, AW, "A")
    UT = ca.bf16(4, S)
    mark_after_UT = ca.o
    winA = ca.bf16(8, 1024)
    xt2 = [ca.f32(D) for _ in range(2)]
    t1t = [ca.bf16(2, 128) for _ in range(2)]
    junkA = ca.f32(D)
    hb = ca.bf16(D)
    hT = ca.bf16(8, 128)
    ufb = ca.bf16(512)
    bsb = [ca.bf16(2, 512) for _ in range(2)]
    ssA = [ca.f32(1) for _ in range(2)]

    w_in_v = w["w_in"][0].rearrange("(kc p) n -> p kc n", p=128)
    load_cast(winA, w_in_v[:, :, 0:1024], "winA", 4)

    UTv = [UT[:, c, :].rearrange("p (n1 n2) -> p n2 n1", n2=128) for c in range(4)]
    xt3 = [xt2[0], xt2[1], ca.f32(D)]
    t1t3 = [t1t[0], t1t[1], ca.bf16(2, 128)]
    ssA3 = [ssA[0], ssA[1], ca.f32(1)]
    hb2 = [hb, ca.bf16(D)]
    hT2 = [hT, ca.bf16(8, 128)]

    def loadA(a):
        sl = a % 3
        m.ld("sp", xt3[sl][0:64, :], xA[2 * a], [], ["xA%d" % sl])
        m.ld("sp", xt3[sl][64:128, :], xA[2 * a + 1], [], ["xA%d" % sl])
        m.ld("sp", t1t3[sl], cst["t1"][a], [], ["t1t%d" % sl])

    def A1(a):
        s3 = a % 3
        sl = a % 2
        if a + 1 < NT:
            loadA(a + 1)
        rmsnorm(xt3[s3], "xA%d" % s3, gbc["g_mix"], "gbc_g_mix", hb2[sl], "hb%d" % sl, junkA, "junkA", ssA3[s3], "ssA%d" % s3)
        yield
        hTp = pbf(0, 8, 128)
        for kc in range(8):
            m.tr(hTp[:, kc, :], hb2[sl][:, kc * 128:(kc + 1) * 128], ident_bf, ["hb%d" % sl, "ident_bf"], ["P0"])
        m.cp("act", hT2[sl], hTp, ["P0"], ["hT%d" % sl])
        yield

    def A2(a):
        s3 = a % 3
        sl = a % 2
        hT_ = hT2[sl]
        kh = "hT%d" % sl
        m.mm(PB[1][:, :], [(hT_[:, kc, :], winA[:, kc, 0:512]) for kc in range(8)], [kh, "winA"], ["P1"])
        m.cp("act", ufb, PB[1][:, :], ["P1"], ["ufb"])
        yield
        m.mm(PB[2][:, :], [(t1t3[s3][:, 0, :], ufb)], ["ufb", "t1t%d" % s3], ["P2"])
        m.mm(PB[3][:, :], [(t1t3[s3][:, 1, :], ufb)], ["ufb", "t1t%d" % s3], ["P3"])
        kb = "bsb%d" % sl
        m.cp("dve", bsb[sl][:, 0, :], PB[2][:, :], ["P2"], [kb])
        m.cp("dve", bsb[sl][:, 1, :], PB[3][:, :], ["P3"], [kb])
        for off in range(2):
            m.ld("sp", BS[:, :, 2 * a + off, :], bsb[sl][64 * off:64 * off + 64, :, :], [kb], ["BS"])
        yield
        up = PB[4 + (a % 2)][:, :].rearrange("p (c t) -> p c t", c=4)
        kp = "P%d" % (4 + (a % 2))
        for c in range(4):
            m.mm(up[:, c, :], [(winA[:, kc, 512 + c * 128:512 + (c + 1) * 128], hT_[:, kc, :]) for kc in range(8)],
                 [kh, "winA"], [kp])
        yield
        for c in range(4):
            for off in range(2):
                m.cp("act" if c % 2 else "dve", UTv[c][:, 2 * a + off, :], up[:, c, 64 * off:64 * off + 64], [kp], ["UT"])
        yield

    def _interleave(g1, g2):
        d1 = d2 = False
        while not (d1 and d2):
            if not d1:
                try:
                    next(g1)
                except StopIteration:
                    d1 = True
            if not d2:
                try:
                    next(g2)
                except StopIteration:
                    d2 = True

    loadA(0)
    _interleave(A1(0), iter(()))
    for a in range(NT):
        _interleave(A2(a), A1(a + 1) if a + 1 < NT else iter(()))
    if stage == 1:
        d_ut = dbg_out("d_ut", [4, 128, S], BF16)
        for c in range(4):
            m.ld("sp", d_ut[c], UT[:, c, :], ["UT"], ["dbg"])
        d_bs = dbg_out("d_bs", [64, 2, 128, 512], BF16)
        m.ld("sp", d_bs, BS, ["BS"], ["dbg"])
        return nc, m, dbgo, w

    m.barrier()
    cb_ = Carve(arena, AW, "B")
    cb_.o = mark_after_UT
    LB = 16
    NB_ = S // LB
    ROT = [cb_.f32(NB_ + 1), cb_.f32(NB_ + 1)]
    iota = cb_.f32(TB + 1)
    wk = [cb_.f32(TB) for _ in range(6)]
    wk = [wk[0], wk[1], wk[2], wk[3], wk[0], wk[2], wk[4], wk[5]]
    Xs = [[[cb_.bf16(NB_ + 2) for _ in range(2)] for _ in range(4)] for _ in range(2)]
    TBt = cb_.bf16(LB, 2, 128)
    TCx = [cb_.bf16(4, LB + 1, 2, 32) for _ in range(2)]
    KT = [cb_.bf16(LB, 128) for _ in range(2)]
    K0acc = cb_.f32(128)
    BsP4 = cb_.f32(4, 2, 128)
    BsP4b = cb_.bf16(4, 2, 128)
    Pt = cb_.f32(2, LB + 1)
    tbB = cb_.f32(8, 128)
    tbO = cb_.bf16(LB, 2, 128)
    ardt = cb_.f32(2, 16); thr16 = cb_.f32(2, 16); r16 = cb_.f32(2, 16)
    p17 = [cb_.f32(LB + 1) for _ in range(6)]
    p17i = cb_.i32(LB + 1)
    tcA = cb_.f32(LB + 1, 16)
    tcB = cb_.f32(LB + 1, 16)
    are = cb_.f32(2, 16); aim = cb_.f32(2, 16); ldt = cb_.f32(2, 16)
    dtv = cb_.f32(2, 16); th = cb_.f32(2, 16); thr = cb_.f32(2, 16); r1 = cb_.f32(2, 16)
    cth = cb_.f32(2, 16); sth = cb_.f32(2, 16); cfr = cb_.f32(2, 16); cfi = cb_.f32(2, 16)
    sm = [cb_.f32(2, 16) for _ in range(6)]
    smi = cb_.i32(2, 16)
    sq = [cb_.f32(2, 16) for _ in range(3)]
    sqi = cb_.i32(2, 16)
    Cc = cb_.f32(2, 2, 16, 16)
    Cq = cb_.f32(2, 2, 16, 16)
    tbA = Cc.rearrange("p a b c d -> p (a b c d)").rearrange("p (t n) -> p t n", n=128)
    dvec = cb_.f32(4)
    tki = cb_.i32(TB + 1)
    tkf = cb_.f32(TB + 1)
    targ = cb_.f32(TB + 1)
    tmk = cb_.f32(TB + 1)
    gb = cb_.bf16(4, 128)
    tkf2 = cb_.f32(TB + 1)

    m.ld("sp", iota, cst["iota"], [], ["iota"])
    m.ld("sp", dvec, w["ssm_d"][0].rearrange("(c p) -> p c", p=128), [], ["dvec"], slow=True)
    for d in range(2):
        m.ld("sp", are[:, d, :], w["ssm_a_re"][0, d].rearrange("(sc h) n -> (h n) sc", h=2), [], ["are"], slow=True)
        m.ld("sp", aim[:, d, :], w["ssm_a_im"][0, d].rearrange("(sc h) n -> (h n) sc", h=2), [], ["aim"], slow=True)
        ldv = w["ssm_log_dt"][0, d].rearrange("(sc h) -> h sc", h=2)
        for h in range(2):
            m.ld("sp", ldt[64 * h:64 * h + 64, d, :], ldv[h:h + 1, :].partition_broadcast(64), [], ["ldt"], slow=True)
            for r, nm in enumerate(("ssm_c_re", "ssm_c_im")):
                cv = w[nm][0, d].rearrange("(sc h) o n -> h n sc o", h=2)
                for sc_ in range(16):
                    m.ld("sp", Cc[64 * h:64 * h + 64, d, r, sc_, :], cv[h][:, sc_, :], [], ["Cc"], slow=True)

    def sincos(arg, karg, n, sin_out, ksin, cos_out, kcos, kf, ki, red, msk):
        ks = "sc_scr"
        m.ts("dve", kf, arg, 1.0 / TWO_PI, None, ALU.mult, None, [karg], [ks])
        m.cp("dve", ki, kf, [ks], [ks])
        m.cp("dve", kf, ki, [ks], [ks])
        m.stt("dve", red, kf, -CW1, arg, ALU.mult, ALU.add, [ks, karg], [ks])
        m.stt("dve", red, kf, -CW2, red, ALU.mult, ALU.add, [ks], [ks])
        for shift, o, ko in ((0.0, sin_out, ksin), (math.pi / 2, cos_out, kcos)):
            m.ts("dve", kf, red, shift, None, ALU.add, None, [ks], [ks])
            m.op("dve", lambda e: e.tensor_single_scalar(msk, kf, math.pi, ALU.is_gt), [ks], [ks])
            m.stt("dve", kf, msk, -TWO_PI, kf, ALU.mult, ALU.add, [ks], [ks])
            m.op("dve", lambda e: e.tensor_single_scalar(msk, kf, -math.pi, ALU.is_lt), [ks], [ks])
            m.stt("dve", kf, msk, TWO_PI, kf, ALU.mult, ALU.add, [ks], [ks])
            m.act(o, kf, ACTF.Sin, [ks], [ko])

    fl = lambda t: t.rearrange("p a b -> p (a b)")
    KP = ["are", "aim", "ldt"]
    m.act(fl(dtv), fl(ldt), ACTF.Exp, ["ldt"], ["dtv"])
    m.tt("dve", fl(th), fl(aim), fl(dtv), ALU.mult, ["aim", "dtv"], ["th"])
    m.tt("dve", fl(ardt), fl(are), fl(dtv), ALU.mult, ["are", "dtv"], ["ardt"])
    m.act(fl(r1), fl(ardt), ACTF.Exp, ["ardt"], ["r1"])
    m.act(fl(r16), fl(ardt), ACTF.Exp, ["ardt"], ["r16"], scale=float(LB))
    sincos(fl(th), "th", 32, fl(sth), "sth", fl(cth), "cth", fl(sq[0]), fl(sqi), fl(sq[1]), fl(sq[2]))
    m.ts("dve", fl(sm[1]), fl(th), 1.0 / TWO_PI, None, ALU.mult, None, ["th"], ["sm1"])
    m.cp("dve", fl(smi), fl(sm[1]), ["sm1"], ["smi"])
    m.cp("dve", fl(sm[1]), fl(smi), ["smi"], ["sm1"])
    m.stt("dve", fl(thr), fl(sm[1]), -CW1, fl(th), ALU.mult, ALU.add, ["sm1", "th"], ["thr"])
    m.stt("dve", fl(thr), fl(sm[1]), -CW2, fl(thr), ALU.mult, ALU.add, ["sm1", "thr"], ["thr"])
    m.tt("dve", fl(sm[0]), fl(r1), fl(cth), ALU.mult, ["r1", "cth"], ["sm0"])
    m.ts("dve", fl(sm[0]), fl(sm[0]), -1.0, None, ALU.add, None, ["sm0"], ["sm0"])
    m.tt("dve", fl(sm[1]), fl(r1), fl(sth), ALU.mult, ["r1", "sth"], ["sm1"])
    m.tt("dve", fl(sm[2]), fl(are), fl(are), ALU.mult, ["are"], ["sm2"])
    m.tt("dve", fl(sm[3]), fl(aim), fl(aim), ALU.mult, ["aim"], ["sm3"])
    m.tt("dve", fl(sm[2]), fl(sm[2]), fl(sm[3]), ALU.add, ["sm2", "sm3"], ["sm2"])
    m.op("dve", lambda e: e.reciprocal(fl(sm[2]), fl(sm[2])), ["sm2"], ["sm2"])
    m.tt("dve", fl(sm[3]), fl(sm[0]), fl(are), ALU.mult, ["sm0", "are"], ["sm3"])
    m.tt("dve", fl(sm[4]), fl(sm[1]), fl(aim), ALU.mult, ["sm1", "aim"], ["sm4"])
    m.tt("dve", fl(sm[3]), fl(sm[3]), fl(sm[4]), ALU.add, ["sm3", "sm4"], ["sm3"])
    m.tt("dve", fl(cfr), fl(sm[3]), fl(sm[2]), ALU.mult, ["sm3", "sm2"], ["cfr"])
    m.tt("dve", fl(sm[3]), fl(sm[1]), fl(are), ALU.mult, ["sm1", "are"], ["sm3"])
    m.tt("dve", fl(sm[4]), fl(sm[0]), fl(aim), ALU.mult, ["sm0", "aim"], ["sm4"])
    m.tt("dve", fl(sm[3]), fl(sm[3]), fl(sm[4]), ALU.subtract, ["sm3", "sm4"], ["sm3"])
    m.tt("dve", fl(cfi), fl(sm[3]), fl(sm[2]), ALU.mult, ["sm3", "sm2"], ["cfi"])
    for d in range(2):
        cr = cfr[:, d, :].unsqueeze(2).to_broadcast([128, 16, 16])
        ci = cfi[:, d, :].unsqueeze(2).to_broadcast([128, 16, 16])
        t0 = wk[0][:, 0:256].rearrange("p (a b) -> p a b", b=16)
        t1_ = wk[1][:, 0:256].rearrange("p (a b) -> p a b", b=16)
        m.tt("dve", t0, Cc[:, d, 0], cr, ALU.mult, ["Cc", "cfr"], ["wk0"])
        m.tt("dve", t1_, Cc[:, d, 1], ci, ALU.mult, ["Cc", "cfi"], ["wk1"])
        m.tt("dve", Cq[:, d, 0], t0, t1_, ALU.subtract, ["wk0", "wk1"], ["Cq"])
        m.tt("dve", t0, Cc[:, d, 0], ci, ALU.mult, ["Cc", "cfi"], ["wk0"])
        m.tt("dve", t1_, Cc[:, d, 1], cr, ALU.mult, ["Cc", "cfr"], ["wk1"])
        m.tt("dve", t0, t0, t1_, ALU.add, ["wk0", "wk1"], ["wk0"])
        m.ts("dve", Cq[:, d, 1], t0, -1.0, None, ALU.mult, None, ["wk0"], ["Cq"])

    pk = [cb_.f32(TB) for _ in range(2)]
    gl = [pk[0], pk[1], wk[3]]
    m.ts("dve", fl(sm[1]), fl(thr), float(LB) / TWO_PI, None, ALU.mult, None, ["thr"], ["sm1"])
    m.cp("dve", fl(smi), fl(sm[1]), ["sm1"], ["smi"])
    m.cp("dve", fl(sm[1]), fl(smi), ["smi"], ["sm1"])
    m.ts("dve", fl(thr16), fl(thr), float(LB), None, ALU.mult, None, ["thr"], ["thr16"])
    m.stt("dve", fl(thr16), fl(sm[1]), -CW1, fl(thr16), ALU.mult, ALU.add, ["sm1", "thr16"], ["thr16"])
    m.stt("dve", fl(thr16), fl(sm[1]), -CW2, fl(thr16), ALU.mult, ALU.add, ["sm1", "thr16"], ["thr16"])
    YSj = YS.rearrange("(q c j) d -> j c q d", j=LB, c=128)
    for d in range(2):
        for scl in range(4):
            for r in range(2):
                m.memset("pool", Xs[d][scl][r], 0.0, ["X%d_%d" % (d, scl)])
    for chunk in range(4):
        UTc = UT[:, chunk, :]
        for d in range(2):
            kT = "TCx%d" % d
            m.memset("pool", TCx[d], 0.0, [kT])
            m.memset("pool", BsP4, 0.0, ["BsP4"])
            for scl in range(4):
                sc = chunk * 4 + scl
                for r, nm in enumerate(("ssm_b_re", "ssm_b_im")):
                    bv = w[nm][0, d].rearrange("(sc h) n i -> sc h n i", h=2)
                    for h in range(2):
                        c0_ = scl * 32 + h * 16
                        m.ld("sp", BsP4[64 * h:64 * h + 64, scl, r, c0_:c0_ + 16], bv[sc, h], [], ["BsP4"])
            m.cp("dve", BsP4b, BsP4, ["BsP4"], ["BsP4b"])
            for scl in range(4):
                sc = chunk * 4 + scl
                i17 = iota[:, 0:LB + 1]
                m.ts("dve", p17[0], i17, thr[:, d, sc:sc + 1], None, ALU.mult, None, ["iota", "thr"], ["p17_0"])
                sincos(p17[0], "p17_0", LB + 1, p17[1], "p17_1", p17[2], "p17_2", p17[3], p17i, p17[4], p17[5])
                m.act(p17[3], i17, ACTF.Exp, ["iota", "ardt", "sc_scr"], ["sc_scr"], scale=ardt[:, d, sc:sc + 1])
                m.tt("dve", Pt[:, 0, :], p17[3], p17[2], ALU.mult, ["sc_scr", "p17_2"], ["Pt"])
                m.tt("dve", Pt[:, 1, :], p17[3], p17[1], ALU.mult, ["sc_scr", "p17_1"], ["Pt"])
                for h in range(2):
                    rows = slice(64 * h, 64 * h + 64)
                    q0 = Cq[rows, d, 0, sc, :].unsqueeze(1).to_broadcast([64, LB + 1, 16])
                    q1 = Cq[rows, d, 1, sc, :].unsqueeze(1).to_broadcast([64, LB + 1, 16])
                    pre = Pt[rows, 0, :].unsqueeze(2).to_broadcast([64, LB + 1, 16])
                    pim = Pt[rows, 1, :].unsqueeze(2).to_broadcast([64, LB + 1, 16])
                    m.tt("pool", tcA[rows], q0, pre, ALU.mult, ["Cq", "Pt"], ["tcA"])
                    m.tt("pool", tcB[rows], q1, pim, ALU.mult, ["Cq", "Pt"], ["tcB"])
                    m.tt("pool", TCx[d][rows, scl, :, 0, 16 * h:16 * h + 16], tcA[rows], tcB[rows], ALU.add, ["tcA", "tcB"], [kT])
                    m.tt("pool", tcA[rows], q1, pre, ALU.mult, ["Cq", "Pt"], ["tcA"])
                    m.tt("pool", tcB[rows], q0, pim, ALU.mult, ["Cq", "Pt"], ["tcB"])
                    m.tt("pool", TCx[d][rows, scl, :, 1, 16 * h:16 * h + 16], tcA[rows], tcB[rows], ALU.subtract, ["tcA", "tcB"], [kT])
                m.memset("pool", tbO, 0.0, ["tbO"])
                for h in range(2):
                    rows = slice(64 * h, 64 * h + 64)
                    cols = slice(scl * 32 + 16 * h, scl * 32 + 16 * h + 16)
                    prev = Pt[rows, 0, 0:LB][:, ::-1].unsqueeze(2).to_broadcast([64, LB, 16])
                    pimv = Pt[rows, 1, 0:LB][:, ::-1].unsqueeze(2).to_broadcast([64, LB, 16])
                    bre = BsP4[rows, scl, 0, cols].unsqueeze(1).to_broadcast([64, LB, 16])
                    bim = BsP4[rows, scl, 1, cols].unsqueeze(1).to_broadcast([64, LB, 16])
                    tA = tbB[rows].rearrange("p a b -> p (a b)")[:, 0:256].rearrange("p (a b) -> p a b", b=16)
                    tB_ = tbB[rows].rearrange("p a b -> p (a b)")[:, 256:512].rearrange("p (a b) -> p a b", b=16)
                    m.tt("dve", tA, bre, prev, ALU.mult, ["BsP4", "Pt"], ["tbB"])
                    m.tt("dve", tB_, bim, pimv, ALU.mult, ["BsP4", "Pt"], ["tbB"])
                    m.tt("dve", tbO[rows, :, 0, cols], tA, tB_, ALU.subtract, ["tbB"], ["tbO"])
                    m.tt("dve", tA, bre, pimv, ALU.mult, ["BsP4", "Pt"], ["tbB"])
                    m.tt("dve", tB_, bim, prev, ALU.mult, ["BsP4", "Pt"], ["tbB"])
                    m.tt("dve", tbO[rows, :, 1, cols], tA, tB_, ALU.add, ["tbB"], ["tbO"])
                for g4 in range(4):
                    tpb = pbf(6 + g4 % 2, 4, 2, 128)
                    kb_ = "P%d" % (6 + g4 % 2)
                    for t4 in range(4):
                        for r in range(2):
                            m.tr(tpb[:, t4, r, :], tbO[:, 4 * g4 + t4, r, :], ident_bf, ["tbO", "ident_bf"], [kb_])
                    m.cp("act", TBt[:, 4 * g4:4 * g4 + 4, :, :], tpb, [kb_], ["TBt"])
                for r in range(2):
                    m.mm(PB[r][:, :], [(TBt[:, (t if d == 0 else LB - 1 - t), r, :], UTc[:, t::LB]) for t in range(LB)], ["TBt", "UT"], ["P%d" % r])
                m.ts("dve", targ, iota, thr16[:, d, sc:sc + 1], None, ALU.mult, None, ["iota", "thr16"], ["targ"])
                sincos(targ, "targ", TB + 1, ROT[1], "rot", ROT[0], "rot", tkf, tki, tmk, tkf2)
                pr = PB[0][:, :] if d == 0 else PB[0][:, ::-1]
                pi_ = PB[1][:, :] if d == 0 else PB[1][:, ::-1]
                COSv = ROT[0][:, 0:NB_]
                SINv = ROT[1][:, 0:NB_]
                m.tt("dve", wk[0], pr, COSv, ALU.mult, ["P0", "rot"], ["wk0"])
                m.tt("dve", wk[1], pi_, SINv, ALU.mult, ["P1", "rot"], ["wk1"])
                m.tt("dve", wk[2], pi_, COSv, ALU.mult, ["P1", "rot"], ["wk2"])
                m.tt("dve", wk[3], pr, SINv, ALU.mult, ["P0", "rot"], ["wk3"])
                m.tt("dve", wk[0], wk[0], wk[1], ALU.add, ["wk0", "wk1"], ["wk0"])
                m.tt("dve", wk[2], wk[2], wk[3], ALU.subtract, ["wk2", "wk3"], ["wk2"])
                r16b = r16[:, d, sc:sc + 1].to_broadcast([128, NB_])
                m.op("dve", (lambda o=wk[6], a=r16b, b_=wk[0]: (lambda e: e.tensor_tensor_scan(o, a, b_, 0.0, ALU.mult, ALU.add)))(), ["wk0", "r16"], ["wk6"])
                m.op("dve", (lambda o=wk[7], a=r16b, b_=wk[2]: (lambda e: e.tensor_tensor_scan(o, a, b_, 0.0, ALU.mult, ALU.add)))(), ["wk2", "r16"], ["wk7"])
                kx_ = "X%d_%d" % (d, scl)
                if d == 0:
                    xre = Xs[d][scl][0][:, 1:NB_ + 1]
                    xim = Xs[d][scl][1][:, 1:NB_ + 1]
                else:
                    xre = Xs[d][scl][0][:, 0:NB_][:, ::-1]
                    xim = Xs[d][scl][1][:, 0:NB_][:, ::-1]
                m.tt("pool", pk[0], wk[6], COSv, ALU.mult, ["wk6", "rot"], ["pk0"])
                m.tt("pool", pk[1], wk[7], SINv, ALU.mult, ["wk7", "rot"], ["pk1"])
                m.tt("pool", xre, pk[0], pk[1], ALU.subtract, ["pk0", "pk1"], [kx_])
                m.tt("pool", pk[0], wk[6], SINv, ALU.mult, ["wk6", "rot"], ["pk0"])
                m.tt("pool", pk[1], wk[7], COSv, ALU.mult, ["wk7", "rot"], ["pk1"])
                m.tt("pool", xim, pk[0], pk[1], ALU.add, ["pk0", "pk1"], [kx_])
            for m_ in range(LB):
                bk = 2 + m_ % 2
                kb_ = "P%d" % bk
                first = True
                for scl in range(4):
                    for r in range(2):
                        m.op("pe", (lambda o=PB[bk][:, scl * 32:scl * 32 + 32], l=BsP4b[:, scl, r, :], rr=TCx[d][:, scl, m_, r, :], st=first, sp_=(scl == 3 and r == 1):
                                    (lambda e: e.matmul(o, l, rr, start=st, stop=sp_)))(), ["BsP4b", kT], [kb_])
                        first = False
                if m_ == 0:
                    if d == 0:
                        m.stt("dve", K0acc, ident_f, dvec[:, chunk:chunk + 1], PB[bk][:, 0:128], ALU.mult, ALU.add, ["ident_f", "dvec", kb_], ["K0acc"])
                    else:
                        m.tt("dve", KT[0][:, 0, :], PB[bk][:, 0:128], K0acc, ALU.add, [kb_, "K0acc"], ["KT"])
                else:
                    m.cp("act", KT[d][:, m_, :], PB[bk][:, 0:128], [kb_], ["KT"])
        for j in range(LB):
            bk = 4 + j % 2
            kb_ = "P%d" % bk
            nmm = 0
            tot = 4 * (LB + 16)
            for q in range(4):
                base = 16 * 128 * q
                for mlag in range(-(LB - 1 - j), j + 1):
                    t_ = j - mlag
                    lhs = UTc[:, base + t_: base + t_ + 16 * 127 + 1: 16]
                    if mlag >= 0:
                        rhs = KT[0][:, mlag, :]
                    else:
                        rhs = KT[1][:, -mlag, :]
                    m.op("pe", (lambda o=PB[bk][:, q * 128:(q + 1) * 128], l=lhs, rr=rhs, st=(nmm == 0), sp_=(nmm == tot - 1):
                                (lambda e: e.matmul(o, l, rr, start=st, stop=sp_)))(), ["UT", "KT"], [kb_])
                    nmm += 1
                for d in range(2):
                    idx = (j + 1) if d == 0 else (LB - j)
                    c0_ = q * 128 if d == 0 else q * 128 + 1
                    for scl in range(4):
                        for r in range(2):
                            m.op("pe", (lambda o=PB[bk][:, q * 128 + scl * 32:q * 128 + scl * 32 + 32], l=Xs[d][scl][r][:, c0_:c0_ + 128], rr=TCx[d][:, scl, idx, r, :], st=(nmm == 0), sp_=(nmm == tot - 1):
                                        (lambda e: e.matmul(o, l, rr, start=st, stop=sp_)))(), ["X%d_%d" % (d, scl), "TCx%d" % d], [kb_])
                            nmm += 1
            assert nmm == tot, (nmm, tot)
            m.cp("dve", gl[0], PB[bk][:, :], [kb_], ["pk0"])
            m.tt("pool", gl[1], gl[0], gl[0], ALU.mult, ["pk0"], ["pk1"])
            m.ts("pool", gl[1], gl[1], 0.044715, 1.0, ALU.mult, ALU.add, ["pk1"], ["pk1"])
            m.tt("pool", gl[1], gl[1], gl[0], ALU.mult, ["pk1", "pk0"], ["pk1"])
            m.act(gl[2], gl[1], ACTF.Sigmoid, ["pk1"], ["wk3"], scale=1.5957691216057308)
            m.tt("dve", gb.rearrange("p q c -> p (q c)"), gl[0], gl[2], ALU.mult, ["pk0", "wk3"], ["gb"])
            m.ld("sp", YSj[j][:, :, chunk * 128:(chunk + 1) * 128], gb, ["gb"], ["YS"])
    if stage == 2:
        d_ys = dbg_out("d_ys", [S, 512], BF16)
        m.ld("sp", d_ys, YS, ["YS"], ["dbg"])
        d_ut = dbg_out("d_ut", [4, 128, S], BF16)
        for c in range(4):
            m.ld("sp", d_ut[c], UT[:, c, :], ["UT"], ["dbg"])
        d_sm = dbg_out("d_sm", [6, 128, 32], F32)
        for i_, (t_, k_) in enumerate(((r1, "r1"), (thr, "thr"), (cfr, "cfr"), (cfi, "cfi"), (cth, "cth"), (sth, "sth"))):
            m.ld("sp", d_sm[i_], fl(t_), [k_], ["dbg"])
        return nc, m, dbgo, w
    m.barrier()
    cc = Carve(arena, AW, "C")
    wing = cc.bf16(8, 2048)
    Wc = cc.bf16(4, 1024)
    Ws = cc.bf16(4, 1024)
    wglu = cc.bf16(4, 2048)
    wout = cc.bf16(8, 1024)
    wrt = cc.f32(8, NE)
    r3 = cc.bf16(2, 256)
    cs128 = cc.f32(2, 128)
    AFFT = cc.f32(S)
    wf32 = AFFT[:, 0:4096].rearrange("p (g n) -> p g n", g=4)
    xtC = [cc.f32(D) for _ in range(2)]
    DtC = [cc.bf16(2, 512) for _ in range(2)]
    hbC = cc.bf16(D)
    hTC = cc.bf16(8, 128)
    G = cc.f32(2048)
    AT = cc.bf16(4, 256)
    ytC2 = [cc.bf16(512) for _ in range(2)]
    ysT = cc.bf16(4, 128)
    sgC = cc.f32(D)
    tvC = cc.f32(D)
    mix = cc.f32(D)
    mbC = cc.bf16(D)
    mTC = cc.bf16(8, 128)
    x1t = cc.f32(D)
    xn = cc.f32(D)
    xnT = cc.f32(8, 128)
    h2b = cc.bf16(D)
    smC = [cc.f32(1) for _ in range(6)]
    lgt = cc.f32(NE)
    ext = cc.f32(NE)

    load_cast(wing, w_in_v[:, :, 1024:3072], "wing", 8)
    load_cast(wglu, w["w_glu"][0].rearrange("(c p) n -> p c n", p=128), "wglu", 4)
    load_cast(wout, w["w_out"][0].rearrange("(c p) n -> p c n", p=128), "wout", 8)
    m.ld("sp", wrt, w["w_router"][0].rearrange("(c p) n -> p c n", p=128), [], ["wrt"])
    m.ld("sp", r3, cst["r3"], [], ["r3"])
    m.ld("sp", cs128, cst["cs128"], [], ["cs128"])
    m.ld("sp", wf32, w["w_fourier"][0].rearrange("(g p) n -> p g n", p=128), [], ["AFFT"])
    for g_ in range(4):
        for half in range(2):
            for r, (Wt, kW) in enumerate(((Wc, "Wc"), (Ws, "Ws"))):
                bk = 1 + (2 * half + r) % 2
                m.mm(PB[bk][:, :], [(cs128[:, r, :], wf32[:, g_, half * 512:(half + 1) * 512])], ["cs128", "AFFT"], ["P%d" % bk])
                m.cp("dve", Wt[:, g_, half * 512:(half + 1) * 512], PB[bk][:, :], ["P%d" % bk], [kW])

    def loadC(j):
        sl = j % 2
        m.ld("sp", xtC[sl], xC[j], [], ["xC%d" % sl])
        m.ld("sp", DtC[sl], BS[j].rearrange("r n c -> n r c"), ["BS"], ["DtC%d" % sl])
        m.ld("sp", ytC2[sl], YSC[j], ["YS"], ["ytC%d" % sl])
    x1t2 = [x1t, cc.f32(D)]

    def main_steps(j):
        sl = j % 2
        kx = "xC%d" % sl
        kd = "DtC%d" % sl
        ytC = ytC2[sl]
        kyt = "ytC%d" % sl
        x1t_ = x1t2[sl]
        kx1 = "x1t%d" % sl
        if j + 1 < NT:
            loadC(j + 1)
        rmsnorm(xtC[sl], kx, gbc["g_mix"], "gbc_g_mix", hbC, "hbC", mix, "mix", smC[0], "smC0")
        hTp = pbf(0, 8, 128)
        for kc in range(8):
            m.tr(hTp[:, kc, :], hbC[:, kc * 128:(kc + 1) * 128], ident_bf, ["hbC", "ident_bf"], ["P0"])
        m.cp("act", hTC, hTp, ["P0"], ["hTC"])
        yield
        for q in range(4):
            bk = 1 + q % 2
            m.mm(PB[bk][:, :], [(hTC[:, kc, :], wing[:, kc, q * 512:(q + 1) * 512]) for kc in range(8)], ["hTC", "wing"], ["P%d" % bk])
            m.act(G[:, q * 512:(q + 1) * 512], PB[bk][:, :], ACTF.Sigmoid, ["P%d" % bk], ["G"])
            if q % 2:
                yield
        for c in range(4):
            bk = 3 + c // 2
            o_ = PB[bk][:, (c % 2) * 256:(c % 2) * 256 + 256]
            m.mm(o_, [(DtC[sl][:, 0, c * 128:(c + 1) * 128], r3[:, 0, :]), (DtC[sl][:, 1, c * 128:(c + 1) * 128], r3[:, 1, :])], [kd, "r3"], ["P%d" % bk])
        m.cp("dve", AT[:, 0:2, :], PB[3][:, :].rearrange("p (c n) -> p c n", c=2), ["P3"], ["AT"])
        m.cp("dve", AT[:, 2:4, :], PB[4][:, :].rearrange("p (c n) -> p c n", c=2), ["P4"], ["AT"])
        ytp = pbf(7, 4, 128)
        for c in range(4):
            m.tr(ytp[:, c, :], ytC[:, c * 128:(c + 1) * 128], ident_bf, [kyt, "ident_bf"], ["P7"])
        m.cp("act", ysT, ytp, ["P7"], ["ysT"])
        yield
        for half in range(2):
            bk = 5 + half
            pairs = []
            for c in range(4):
                pairs.append((AT[:, c, 0:128], Wc[:, c, half * 512:(half + 1) * 512]))
                pairs.append((AT[:, c, 128:256], Ws[:, c, half * 512:(half + 1) * 512]))
            m.mm(PB[bk][:, :], pairs, ["AT", "Wc", "Ws"], ["P%d" % bk])
            m.tt("dve", mix[:, half * 512:(half + 1) * 512], PB[bk][:, :], G[:, half * 512:(half + 1) * 512], ALU.mult, ["P%d" % bk, "G"], ["mix"])
        yield
        for half in range(2):
            bg = 5 + half
            bv = 1 + half
            hs = slice(half * 512, (half + 1) * 512)
            m.mm(PB[bg][:, :], [(ysT[:, c, :], wglu[:, c, 1024 + half * 512:1024 + (half + 1) * 512]) for c in range(4)], ["ysT", "wglu"], ["P%d" % bg])
            m.act(sgC[:, hs], PB[bg][:, :], ACTF.Sigmoid, ["P%d" % bg], ["sgC"])
            m.mm(PB[bv][:, :], [(ysT[:, c, :], wglu[:, c, half * 512:(half + 1) * 512]) for c in range(4)], ["ysT", "wglu"], ["P%d" % bv])
            m.tt("dve", tvC[:, hs], PB[bv][:, :], sgC[:, hs], ALU.mult, ["P%d" % bv, "sgC"], ["tvC"])
            m.tt("pool", tvC[:, hs], tvC[:, hs], G[:, 1024 + half * 512:1024 + (half + 1) * 512], ALU.mult, ["tvC", "G"], ["tvC"])
            m.tt("dve", mbC[:, hs], mix[:, hs], tvC[:, hs], ALU.add, ["mix", "tvC"], ["mbC"])
            yield
        mTp = pbf(0, 8, 128)
        for kc in range(8):
            m.tr(mTp[:, kc, :], mbC[:, kc * 128:(kc + 1) * 128], ident_bf, ["mbC", "ident_bf"], ["P0"])
        m.cp("act", mTC, mTp, ["P0"], ["mTC"])
        yield
        for half in range(2):
            bk = 3 + half
            hs = slice(half * 512, (half + 1) * 512)
            m.mm(PB[bk][:, :], [(mTC[:, kc, :], wout[:, kc, hs]) for kc in range(8)], ["mTC", "wout"], ["P%d" % bk])
            m.tt("dve", x1t_[:, hs], PB[bk][:, :], xtC[sl][:, hs], ALU.add, ["P%d" % bk, kx], [kx1])
        m.ld("sp", X1C[j], x1t_, [kx1], ["X1"])
        yield

    def tail_steps(j):
        sl = j % 2
        x1t_ = x1t2[sl]
        kx1 = "x1t%d" % sl
        rmsnorm(x1t_, kx1, gbc["g_ffn"], "gbc_g_ffn", xn, "xn", xn, "xn", smC[1], "smC1")
        m.cp("act", h2b, xn, ["xn"], ["h2b"])
        m.ld("sp", H2C[j], h2b, ["h2b"], ["H2"])
        yield
        for hh in range(2):
            tpv = PB[7][:, :].rearrange("p (c n) -> p c n", c=4)
            for k4 in range(4):
                kc = hh * 4 + k4
                m.tr(tpv[:, k4, :], xn[:, kc * 128:(kc + 1) * 128], ident_f, ["xn", "ident_f"], ["P7"])
            m.cp("act" if hh else "dve", xnT[:, hh * 4:(hh + 1) * 4, :], tpv, ["P7"], ["xnT"])
            yield
        m.mm(PB[7][:, 0:NE], [(xnT[:, kc, :], wrt[:, kc, :]) for kc in range(8)], ["xnT", "wrt"], ["P7"])
        m.cp("dve", lgt, PB[7][:, 0:NE], ["P7"], ["lgt"])
        m.op("dve", lambda e: e.tensor_reduce(smC[2], lgt, mybir.AxisListType.X, ALU.max), ["lgt"], ["smC2"])
        m.ts("dve", smC[3], smC[2], -1.0, None, ALU.mult, None, ["smC2"], ["smC3"])
        yield
        m.act(ext, lgt, ACTF.Exp, ["lgt", "smC3"], ["ext", "smC4"], bias=smC[3][:, 0:1], accum=smC[4])
        m.op("dve", lambda e: e.reciprocal(smC[5], smC[4]), ["smC4"], ["smC5"])
        m.ts("dve", AFF_TM[:, j, :], ext, smC[5][:, 0:1], None, ALU.mult, None, ["ext", "smC5"], ["AFF_TM"])
        yield
        m.tr(PB[7][0:NE, 128:256], AFF_TM[:, j, :], ident_f, ["AFF_TM", "ident_f"], ["P7"])
        m.cp("dve", AFFT[0:NE, j * 128:(j + 1) * 128], PB[7][0:NE, 128:256], ["P7"], ["AFFT"])
        yield

    def interleave(g1, g2):
        d1 = d2 = False
        while not (d1 and d2):
            if not d1:
                try:
                    next(g1)
                except StopIteration:
                    d1 = True
            if not d2:
                try:
                    next(g2)
                except StopIteration:
                    d2 = True

    loadC(0)
    for j in range(NT):
        interleave(main_steps(j), tail_steps(j - 1) if j > 0 else iter(()))
    interleave(tail_steps(NT - 1), iter(()))
    m.ld("sp", AFs, AFFT[0:NE, :], ["AFFT"], ["AFs"])
    if stage == 3:
        d_x1 = dbg_out("d_x1", [S, D], F32)
        m.ld("sp", d_x1, X1, ["X1"], ["dbg"])
        d_aff = dbg_out("d_aff", [NE, S], F32)
        m.ld("sp", d_aff, AFs, ["AFs"], ["dbg"])
        return nc, m, dbgo, w
    m.barrier()
    cd_ = Carve(arena, AW, "D")
    A8 = cd_.f32(1024)
    junkD = cd_.f32(1024)
    Mk = cd_.f32(1024)
    cum = cd_.f32(1024)
    idxf = cd_.f32(1024)
    POSF = cd_.f32(NT, NE)
    blk8 = cd_.f32(128)
    tri8 = cd_.f32(128)
    erow = cd_.f32(1)
    lo = cd_.f32(1); hi = cd_.f32(1); mid = cd_.f32(1); ge = cd_.f32(1); d1 = cd_.f32(1); d2 = cd_.f32(1); c0 = cd_.f32(1)
    cnt2 = cd_.f32(2)
    m.ld("sp", A8, AFs.rearrange("e (s c) -> (e s) c", s=8), ["AFs"], ["A8"])
    m.ld("sp", blk8, cst["blk8"], [], ["blk8"])
    m.ld("sp", tri8, cst["tri8"], [], ["tri8"])
    m.ld("sp", erow, cst["erow"], [], ["erow"])
    m.memset("dve", lo, 0.0, ["lo"])
    m.memset("dve", hi, 1.0, ["hi"])
    m.memset("dve", cnt2, 0.0, ["cnt2"])
    for it in range(36):
        m.tt("dve", mid, lo, hi, ALU.add, ["lo", "hi"], ["mid"])
        m.ts("dve", mid, mid, 0.5, None, ALU.mult, None, ["mid"], ["mid"])
        m.ts("dve", junkD, A8, mid[:, 0:1], None, ALU.is_gt, ALU.add, ["A8", "mid"], ["junkD", "cnt2"], accum=cnt2[:, 0:1])
        m.mm(PB[0][:, 0:2], [(blk8, cnt2)], ["blk8", "cnt2"], ["P0"])
        m.op("dve", lambda e: e.tensor_single_scalar(ge, PB[0][:, 0:1], CAP - 0.5, ALU.is_gt), ["P0"], ["ge"])
        m.tt("dve", d1, mid, lo, ALU.subtract, ["mid", "lo"], ["d1"])
        m.stt("dve", lo, d1, ge[:, 0:1], lo, ALU.mult, ALU.add, ["d1", "ge", "lo"], ["lo"])
        m.tt("dve", d2, hi, mid, ALU.subtract, ["hi", "mid"], ["d2"])
        m.stt("dve", hi, d2, ge[:, 0:1], mid, ALU.mult, ALU.add, ["d2", "ge", "mid"], ["hi"])
    m.ts("dve", Mk, A8, lo[:, 0:1], None, ALU.is_gt, None, ["A8", "lo"], ["Mk"])
    m.op("dve", lambda e: e.tensor_tensor_scan(cum, ones_t[:, 0:1].to_broadcast([128, 1024]), Mk, 0.0, ALU.mult, ALU.add), ["Mk", "ones"], ["cum"])
    m.cp("dve", cnt2[:, 0:1], cum[:, 1023:1024], ["cum"], ["cnt2"])
    m.mm(PB[0][:, 2:4], [(tri8, cnt2)], ["tri8", "cnt2"], ["P0"])
    m.tt("dve", c0, PB[0][:, 2:3], erow, ALU.add, ["P0", "erow"], ["c0"])
    m.stt("dve", idxf, cum, c0[:, 0:1], Mk, ALU.add, ALU.mult, ["cum", "c0", "Mk"], ["idxf"])
    m.ts("dve", idxf, idxf, BIG, None, ALU.add, None, ["idxf"], ["idxf"])
    POSIv = POSI.rearrange("p (s c) e -> p s c e", s=8)
    POSFv = POSF.rearrange("p (s c) e -> p s c e", s=8)
    for cb in range(8):
        bk = 1 + cb % 2
        m.tr(PB[bk][:, 0:128], idxf[:, cb * 128:(cb + 1) * 128], ident_f, ["idxf", "ident_f"], ["P%d" % bk])
        src = PB[bk][:, 0:128].rearrange("p (e s) -> p s e", s=8)
        m.cp("dve", POSIv[:, :, cb, :], src, ["P%d" % bk], ["POSI"])
        m.cp("dve", POSFv[:, :, cb, :], src, ["P%d" % bk], ["POSF"])
    m.stt("dve", VALM.rearrange("p a b -> p (a b)"), POSF.rearrange("p a b -> p (a b)"), 0.5 * BIG, AFF_TM.rearrange("p a b -> p (a b)"),
          ALU.is_lt, ALU.mult, ["POSF", "AFF_TM"], ["VALM"])
    if stage == 4:
        d_pos = dbg_out("d_pos", [128, NT * NE], F32)
        m.ld("sp", d_pos, POSF.rearrange("p a b -> p (a b)"), ["POSF"], ["dbg"])
        d_val = dbg_out("d_val", [128, NT * NE], F32)
        m.ld("sp", d_val, VALM.rearrange("p a b -> p (a b)"), ["VALM"], ["dbg"])
        return nc, m, dbgo, w
    m.barrier()
    ce = Carve(arena, AW, "E")
    h2t = [ce.bf16(D) for _ in range(4)]
    for j in range(NT):
        sl = j % 4
        kh = "h2t%d" % sl
        m.ld("sp", h2t[sl], H2C[j], ["H2"], [kh])
        for e_ in range(ESPLIT):
            m.dma("pool", (lambda src=h2t[sl], ix=POSI[:, j, e_:e_ + 1]: (lambda e: e.indirect_dma_start(
                out=XG, out_offset=bass.IndirectOffsetOnAxis(ap=ix, axis=0), in_=src, in_offset=None,
                bounds_check=_bc(e), oob_is_err=False)))(), [kh, "POSI"], ["XGw%d" % (j * NE + e_)])
    m.barrier()
    cf = Carve(arena, AW, "F")
    xgt = [cf.bf16(D) for _ in range(2)]
    xgT = cf.bf16(8, CAP)
    wd = cf.bf16(NFC, D)
    wg = [cf.bf16(8, 256) for _ in range(2)]
    wu = [cf.bf16(8, 256) for _ in range(2)]
    hidT = cf.bf16(NFC, CAP)
    sil = [cf.f32(512) for _ in range(2)]
    yo = [cf.bf16(D) for _ in range(2)]
    nE = NE if stage != 5 else 1
    cnt_i = 0
    stg_g = cf.f32(8, 256)
    stg_u = cf.f32(8, 256)
    stg_d = cf.f32(2, D)
    h2u = [cf.bf16(D) for _ in range(3)]
    for j in range(NT):
        sl = j % 3
        kh = "h2u%d" % sl
        m.ld("pool", h2u[sl], H2C[j], ["H2"], [kh])
        for e_ in range(ESPLIT, NE):
            m.dma("pool", (lambda src=h2u[sl], ix=POSI[:, j, e_:e_ + 1]: (lambda e: e.indirect_dma_start(
                out=XG, out_offset=bass.IndirectOffsetOnAxis(ap=ix, axis=0), in_=src, in_offset=None,
                bounds_check=_bc(e), oob_is_err=False)))(), [kh, "POSI"], ["XGw%d" % (j * NE + e_)])
    xgT2 = [xgT, cf.bf16(8, CAP)]

    def xg_prep(ee):
        dstT = xgT2[ee % 2]
        for ct in range(8):
            sl = ct % 2
            kx = "xgt%d" % sl
            m.ld("sp", xgt[sl], XG[ee * CAP + ct * 128:ee * CAP + (ct + 1) * 128, :], ["XGw"], [kx])
            tp = pbf(0, 8, 128)
            for kc in range(8):
                m.tr(tp[:, kc, :], xgt[sl][:, kc * 128:(kc + 1) * 128], ident_bf, [kx, "ident_bf"], ["P0"])
            m.cp("act" if ct % 2 else "dve", dstT[:, :, ct * 128:(ct + 1) * 128], tp, ["P0"], ["xgT%d" % (ee % 2)])
    for e_ in range(nE):
        wdv = w["w_exp_down"][0, e_].rearrange("(fc p) d -> p fc d", p=128)
        if e_ == ESPLIT and nE > ESPLIT:
            m.barrier()
        if e_ == 0 or e_ == ESPLIT:
            xg_prep(e_)
        xgT = xgT2[e_ % 2]
        kxT = "xgT%d" % (e_ % 2)
        wgv = w["w_exp_gate"][0, e_].rearrange("(kc p) f -> p kc f", p=128)
        wuv = w["w_exp_up"][0, e_].rearrange("(kc p) f -> p kc f", p=128)
        for pc_ in range(NFC // 2):
            sl = pc_ % 2
            fs = slice(pc_ * 256, (pc_ + 1) * 256)
            m.ld("sp", stg_g, wgv[:, :, fs], [], ["stg_g"])
            m.cp("dve", wg[sl], stg_g, ["stg_g"], ["wg%d" % sl])
            m.ld("sp", stg_u, wuv[:, :, fs], [], ["stg_u"])
            m.cp("act", wu[sl], stg_u, ["stg_u"], ["wu%d" % sl])
            m.ld("sp", stg_d, wdv[:, 2 * pc_:2 * pc_ + 2, :], [], ["stg_d"])
            m.cp("dve", wd[:, 2 * pc_:2 * pc_ + 2, :], stg_d, ["stg_d"], ["wd"])
            for fcl in range(2):
                fc = 2 * pc_ + fcl
                for half in range(2):
                    bg = 1 + cnt_i % 2
                    bu = 3 + cnt_i % 2
                    ss_ = cnt_i % 2
                    cnt_i += 1
                    hs = slice(half * 512, (half + 1) * 512)
                    m.mm(PB[bg][:, :], [(wg[sl][:, kc, fcl * 128:(fcl + 1) * 128], xgT[:, kc, hs]) for kc in range(8)], ["wg%d" % sl, kxT], ["P%d" % bg])
                    m.mm(PB[bu][:, :], [(wu[sl][:, kc, fcl * 128:(fcl + 1) * 128], xgT[:, kc, hs]) for kc in range(8)], ["wu%d" % sl, kxT], ["P%d" % bu])
                    m.act(sil[ss_], PB[bg][:, :], ACTF.Silu, ["P%d" % bg], ["sil%d" % ss_])
                    m.tt("dve", hidT[:, fc, hs], PB[bu][:, :], sil[ss_], ALU.mult, ["P%d" % bu, "sil%d" % ss_], ["hidT"])
        if e_ + 1 < nE and e_ + 1 != ESPLIT:
            xg_prep(e_ + 1)
        for ct in range(8):
            sl = ct % 2
            for dh in range(2):
                bk = 5 + dh
                hs = slice(dh * 512, (dh + 1) * 512)
                m.mm(PB[bk][:, :], [(hidT[:, fc, ct * 128:(ct + 1) * 128], wd[:, fc, hs]) for fc in range(NFC)], ["hidT", "wd"], ["P%d" % bk])
                m.cp("act" if dh else "dve", yo[sl][:, hs], PB[bk][:, :], ["P%d" % bk], ["yo%d" % sl])
            m.ld("act", YE[e_ * CAP + ct * 128:e_ * CAP + (ct + 1) * 128, :], yo[sl], ["yo%d" % sl], ["YEw%d" % (e_ * 8 + ct)])
    if stage == 5:
        d_ye = dbg_out("d_ye", [CAP, D], BF16)
        m.ld("sp", d_ye, YE[0:CAP, :], ["YEw"], ["dbg"])
        d_xg = dbg_out("d_xg", [CAP, D], BF16)
        m.ld("sp", d_xg, XG[0:CAP, :], ["XGw"], ["dbg"])
        d_pos = dbg_out("d_pos", [128, NT * NE], F32)
        m.ld("sp", d_pos, POSF.rearrange("p a b -> p (a b)"), [], ["dbg"])
        return nc, m, dbgo, w
    m.barrier()
    cg = Carve(arena, AW, "G")
    wpg = cg.bf16(8, D)
    wpp = cg.bf16(2, D)
    x1g = [cg.f32(D) for _ in range(2)]
    NYB = 8
    ybuf = [cg.bf16(D) for _ in range(NYB)]
    dg = [cg.bf16(128) for _ in range(4)]
    ptt = [cg.f32(256) for _ in range(2)]
    ptb = cg.bf16(256)
    pTg = cg.bf16(2, 128)
    hbg = cg.bf16(D)
    hTg = cg.bf16(8, 128)
    sgg = cg.f32(D)
    jg = cg.f32(D)
    x3 = cg.f32(D)
    og = [cg.f32(D) for _ in range(2)]
    smg = [cg.f32(1) for _ in range(4)]
    load_cast(wpg, w["w_ple_gate"][0].rearrange("(c p) n -> p c n", p=128), "wpg", 8)
    load_cast(wpp, w["w_ple_proj"][0].rearrange("(c p) n -> p c n", p=128), "wpp", 2)
    for b_ in range(NYB):
        m.memset("dve", ybuf[b_], 0.0, ["ybuf%d" % b_])
    gi = 0
    x1g = [x1g[0], x1g[1], cg.f32(D)]
    ptt = [ptt[0], ptt[1], cg.f32(256)]
    gstate = {"gi": 0}

    def loadG(j):
        sl = j % 3
        m.ld("sp", x1g[sl], X1C[j], ["X1"], ["x1g%d" % sl])
        m.ld("sp", ptt[sl], pC[j], [], ["ptt%d" % sl])

    def part1(j):
        sl = j % 3
        kx = "x1g%d" % sl
        accb = (5, 6) if j % 2 == 0 else (2, 4)
        if j + 1 < NT:
            loadG(j + 1)
        for e_ in range(NE):
            gi = gstate["gi"]
            b_ = gi % NYB
            dsl = gi % 4
            gstate["gi"] += 1
            m.dma("pool", (lambda dst=ybuf[b_], ix=POSI[:, j, e_:e_ + 1]: (lambda e: e.indirect_dma_start(
                out=dst, out_offset=None, in_=YE, in_offset=bass.IndirectOffsetOnAxis(ap=ix, axis=0),
                bounds_check=_bc(e), oob_is_err=False)))(), ["YEw", "POSI"], ["ybuf%d" % b_])
            m.ts("dve", dg[dsl], ident_bf, VALM[:, j, e_:e_ + 1], None, ALU.mult, None, ["ident_bf", "VALM"], ["dg%d" % dsl])
            for half in range(2):
                hs = slice(half * 512, (half + 1) * 512)
                m.op("pe", (lambda o=PB[accb[half]][:, :], l=dg[dsl], r=ybuf[b_][:, hs], st=(e_ == 0), sp_=(e_ == NE - 1):
                            (lambda e: e.matmul(o, l, r, start=st, stop=sp_)))(), ["dg%d" % dsl, "ybuf%d" % b_], ["P%d" % accb[half]])
            if e_ % 2:
                yield
        for half in range(2):
            hs = slice(half * 512, (half + 1) * 512)
            m.tt("dve", x1g[sl][:, hs], PB[accb[half]][:, :], x1g[sl][:, hs], ALU.add, ["P%d" % accb[half], kx], [kx])
        yield

    def part2(j):
        sl = j % 3
        kx = "x1g%d" % sl
        m.cp("act", ptb, ptt[sl], ["ptt%d" % sl], ["ptb"])
        ptp = pbf(0, 2, 128)
        for c in range(2):
            m.tr(ptp[:, c, :], ptb[:, c * 128:(c + 1) * 128], ident_bf, ["ptb", "ident_bf"], ["P0"])
        m.cp("act", pTg, ptp, ["P0"], ["pTg"])
        yield
        rmsnorm(x1g[sl], kx, gbc["g_ple"], "gbc_g_ple", hbg, "hbg", jg, "jg", smg[0], "smg0")
        yield
        hp = pbf(7, 8, 128)
        for kc in range(8):
            m.tr(hp[:, kc, :], hbg[:, kc * 128:(kc + 1) * 128], ident_bf, ["hbg", "ident_bf"], ["P7"])
        m.cp("act", hTg, hp, ["P7"], ["hTg"])
        yield
        for half in range(2):
            hs = slice(half * 512, (half + 1) * 512)
            m.mm(PB[1][:, :], [(pTg[:, c, :], wpp[:, c, hs]) for c in range(2)], ["pTg", "wpp"], ["P1"])
            m.mm(PB[3][:, :], [(hTg[:, kc, :], wpg[:, kc, hs]) for kc in range(8)], ["hTg", "wpg"], ["P3"])
            m.act(sgg[:, hs], PB[3][:, :], ACTF.Sigmoid, ["P3"], ["sgg"])
            m.tt("dve", sgg[:, hs], PB[1][:, :], sgg[:, hs], ALU.mult, ["P1", "sgg"], ["sgg"])
            m.tt("dve", x3[:, hs], sgg[:, hs], x1g[sl][:, hs], ALU.add, ["sgg", kx], ["x3"])
            yield
        osl = j % 2
        rmsnorm(x3, "x3", gbc["g_final"], "gbc_g_final", og[osl], "og%d" % osl, jg, "jg", smg[1], "smg1")
        m.ld("sp", outC[j], og[osl], ["og%d" % osl], ["out"])
        yield

    def interleave2(g1, g2):
        d1 = d2 = False
        while not (d1 and d2):
            if not d1:
                try:
                    next(g1)
                except StopIteration:
                    d1 = True
            if not d2:
                try:
                    next(g2)
                except StopIteration:
                    d2 = True

    loadG(0)
    interleave2(part1(0), iter(()))
    for j in range(NT):
        interleave2(part2(j), part1(j + 1) if j + 1 < NT else iter(()))
    return nc, m, dbgo, w


def kernel(**inputs):
    nb = inputs["x"].shape[0]
    nc, m, _, wused = build(stage=99)
    m.finalize(final_wait_keys=["out"])
    cm = host_consts()
    in_maps = []
    for b in range(nb):
        im = {"x": np.ascontiguousarray(inputs["x"][b]), "p": np.ascontiguousarray(inputs["p"][0, b])}
        for n in wused:
            im[n] = np.ascontiguousarray(inputs[n])
        im.update(cm)
        in_maps.append(im)
    res = run_bass_kernel_spmd(nc, in_maps, core_ids=list(range(nb)))
    return np.stack([r["out"] for r in res.results], axis=0).astype(np.float32)
```

```python
import math
import contextlib
import numpy as np
import ml_dtypes
import concourse.bass as bass
import concourse.mybir as mybir
from concourse.bass_utils import run_bass_kernel_spmd

F32 = mybir.dt.float32
BF16 = mybir.dt.bfloat16
I32 = mybir.dt.int32
ALU = mybir.AluOpType
ACTF = mybir.ActivationFunctionType

S = 8192
D = 1024
NT = 64
NE = 16
CAP = 1024
DEXP = 2816
NFC = DEXP // 128
TB = 512
NBLK = S // TB
BIG = 1.0e6
TWO_PI = 2 * math.pi
CW1 = 6.28125
CW2 = float(TWO_PI - 6.28125)
NDS = 48


class MK:
    def __init__(self, nc):
        self.nc = nc
        self.ops = []
        self.es = contextlib.ExitStack()

    def sb(self, name, shape, dt=F32):
        return self.es.enter_context(self.nc.sbuf_tensor(name, list(shape), dt))

    def ps(self, name, shape, dt=F32):
        return self.es.enter_context(self.nc.psum_tensor(name, list(shape), dt))

    def dram(self, name, shape, dt, kind="Internal"):
        return self.nc.dram_tensor(name, list(shape), dt, kind=kind).ap()

    def op(self, eng, emit, R=(), W=()):
        self.ops.append([eng, emit, tuple(R), tuple(W), False])

    def dma(self, eng, emit, R=(), W=()):
        self.ops.append([eng, emit, tuple(R), tuple(W), True])

    def barrier(self):
        self.ops.append(None)

    def tt(self, eng, out, a, b, op, R, W):
        self.op(eng, lambda e: e.tensor_tensor(out, a, b, op), R, W)

    def ts(self, eng, out, a, s1, s2, op0, op1, R, W, accum=None):
        if op1 is None:
            self.op(eng, lambda e: e.tensor_scalar(out, a, s1, None, op0), R, W)
        elif accum is None:
            self.op(eng, lambda e: e.tensor_scalar(out, a, s1, s2, op0, op1), R, W)
        else:
            self.op(eng, lambda e: e.tensor_scalar(out, a, s1, s2, op0, op1, accum_out=accum), R, W)

    def stt(self, eng, out, a, s, b, op0, op1, R, W):
        self.op(eng, lambda e: e.scalar_tensor_tensor(out, a, s, b, op0, op1), R, W)

    def cp(self, eng, out, a, R, W):
        if eng == "act":
            self.op(eng, lambda e: e.copy(out, a), R, W)
        else:
            self.op(eng, lambda e: e.tensor_copy(out, a), R, W)

    def act(self, out, a, func, R, W, bias=None, scale=None, accum=None):
        kw = {}
        if bias is not None:
            kw["bias"] = bias
        if scale is not None:
            kw["scale"] = scale
        if accum is not None:
            kw["accum_out"] = accum
        self.op("act", lambda e: e.activation(out, a, func, **kw), R, W)

    def memset(self, eng, out, val, W):
        self.op(eng, lambda e: e.memset(out, val), (), W)

    def mm(self, out, pairs, R, W):
        n = len(pairs)
        for i, (l, r) in enumerate(pairs):
            self.op("pe", (lambda l=l, r=r, i=i: (lambda e: e.matmul(out, l, r, start=(i == 0), stop=(i == n - 1))))(), R, W)

    def tr(self, out, a, ident, R, W):
        self.op("pe", lambda e: e.transpose(out, a, ident), R, W)

    def ld(self, eng, out, a, R, W, slow=False):
        if slow:
            self.dma(eng, lambda e: e.dma_start(out=out, in_=a, allow_slow_non_contiguous=True), R, W)
        else:
            self.dma(eng, lambda e: e.dma_start(out=out, in_=a), R, W)

    def finalize(self, final_wait_keys=()):
        nc = self.nc
        engs = ["pe", "act", "dve", "pool", "sp"]
        sems = {e: self.es.enter_context(nc.semaphore("sem_" + e)) for e in engs}
        dsems = [self.es.enter_context(nc.semaphore("dsem%d" % i)) for i in range(84)]
        last_w = {}
        readers = {}
        cnt = {e: 0 for e in engs}
        dlast = {}
        ndma = 0
        qcnt = {}
        plan = {e: [] for e in engs}
        pend = {e: [] for e in engs}
        ev = []
        for i, o in enumerate(self.ops):
            if o is None:
                ev.append(None)
                bw = [("c", e, cnt[e]) for e in engs if cnt[e] > 0] + [("d", k, v) for k, v in dlast.items()]
                for e in engs:
                    pend[e] = list(bw)
                continue
            eng, emit, R, W, is_dma = o
            deps = set()
            for r in R:
                if r in last_w:
                    deps.add(last_w[r])
            for w in W:
                if w in last_w:
                    deps.add(last_w[w])
                for rd in readers.get(w, ()):
                    deps.add(rd)
            deps.discard(i)
            waits = [ev[d] for d in deps]
            if is_dma:
                qb, qn = {"sp": (0, 36), "pool": (36, 36)}.get(eng, (72, 12))
                qc = qcnt.get(eng, 0)
                qcnt[eng] = qc + 1
                k = qb + qc % qn
                val = 16 * (qc // qn + 1)
                ndma += 1
                e_ = ("d", k, val)
                dlast[k] = val
                if val > 16:
                    waits.append(("d", k, val - 16))
            else:
                cnt[eng] += 1
                e_ = ("c", eng, cnt[eng])
            ev.append(e_)
            if pend[eng]:
                waits = waits + pend[eng]
                pend[eng] = []
            plan[eng].append((emit, waits, e_))
            for r in R:
                readers.setdefault(r, []).append(i)
            for w in W:
                last_w[w] = i
                readers[w] = []
        final_waits = [ev[last_w[k]] for k in final_wait_keys if k in last_w]

        def run_engine(engname, engobj):
            known = {}

            def do_wait(w):
                kind, a, v = w
                key = (kind, a)
                if known.get(key, 0) >= v:
                    return
                if kind == "c":
                    if a == engname and engname == "pe":
                        return
                    engobj.wait_ge(sems[a], v)
                else:
                    engobj.wait_ge(dsems[a], v)
                known[key] = v

            for emit, waits, e_ in plan[engname]:
                for w in waits:
                    do_wait(w)
                ins = emit(engobj)
                if e_[0] == "c":
                    ins.then_inc(sems[e_[1]], 1)
                else:
                    ins.then_inc(dsems[e_[1]], 16)
            if engname == "sp":
                for w in final_waits:
                    do_wait(w)

        with nc.Block() as block:
            @block.tensor
            def _(e):
                run_engine("pe", e)

            @block.scalar
            def _(e):
                run_engine("act", e)

            @block.vector
            def _(e):
                run_engine("dve", e)

            @block.gpsimd
            def _(e):
                run_engine("pool", e)

            @block.sync
            def _(e):
                run_engine("sp", e)
        self.es.close()
        return {e: len(plan[e]) for e in engs}


class Carve:
    def __init__(self, arena, width, tag):
        self.a = arena
        self.o = 0
        self.w = width
        self.tag = tag
        self.n = 0

    def _take(self, nf):
        assert self.o + nf <= self.w, (self.tag, self.o, nf, self.w)
        v = self.a[:, self.o:self.o + nf]
        self.o += nf
        self.n += 1
        return v

    @staticmethod
    def _shape(v, shape):
        if len(shape) == 1:
            return v
        names = "abcd"[:len(shape)]
        kw = {names[i]: shape[i] for i in range(1, len(shape))}
        return v.rearrange("p (%s) -> p %s" % (" ".join(names), " ".join(names)), **kw)

    def f32(self, *shape):
        n = int(np.prod(shape))
        return self._shape(self._take(n), shape)

    def bf16(self, *shape):
        n = int(np.prod(shape))
        v = self._take((n + 1) // 2).bitcast(BF16)[:, 0:n]
        return self._shape(v, shape)

    def i32(self, *shape):
        n = int(np.prod(shape))
        return self._shape(self._take(n).bitcast(I32), shape)


def host_consts():
    c = {}
    c["ident_bf"] = np.eye(128, dtype=np.float32).astype(ml_dtypes.bfloat16)
    c["ident_f"] = np.eye(128, dtype=np.float32)
    a = np.arange(64)[:, None, None, None, None]
    off = np.arange(2)[None, :, None, None, None]
    n1 = np.arange(64)[None, None, :, None, None]
    off2 = np.arange(2)[None, None, None, :, None]
    k1 = np.arange(64)[None, None, None, None, :]
    tok = 128 * n1 + 2 * a + off
    ang = (2 * np.pi / S) * ((k1 * tok) % S).astype(np.float64)
    delta = (off == off2).astype(np.float64)
    tre = (np.cos(ang) * delta).reshape(64, 128, 128)
    tim = (-np.sin(ang) * delta).reshape(64, 128, 128)
    c["t1"] = np.stack([tre, tim], axis=2).astype(np.float32).astype(ml_dtypes.bfloat16)
    n2 = np.arange(128)[:, None]
    k2 = np.arange(128)[None, :]
    a3 = (2 * np.pi / 128) * ((n2 * k2) % 128)
    cs, sn = np.cos(a3) / 1024.0, np.sin(a3) / 1024.0
    r3 = np.stack([np.concatenate([cs, -sn], axis=1), np.concatenate([sn, cs], axis=1)], axis=1)
    c["r3"] = r3.astype(np.float32).astype(ml_dtypes.bfloat16)
    c["cs128"] = np.stack([np.cos(a3), np.sin(a3)], axis=1).astype(np.float32)
    c["iota"] = np.broadcast_to(np.arange(TB + 1, dtype=np.float32)[None, :], (128, TB + 1)).copy()
    p = np.arange(128)
    same = (p[:, None] // 8) == (p[None, :] // 8)
    c["blk8"] = same.astype(np.float32)
    c["tri8"] = (same & (p[:, None] < p[None, :])).astype(np.float32)
    c["erow"] = ((p // 8) * CAP - 1 - BIG).astype(np.float32).reshape(128, 1)
    return c


CONST_SPECS = [("ident_bf", [128, 128], BF16), ("ident_f", [128, 128], F32), ("t1", [64, 128, 2, 128], BF16),
               ("r3", [128, 2, 256], BF16), ("cs128", [128, 2, 128], F32), ("iota", [128, TB + 1], F32),
               ("blk8", [128, 128], F32), ("tri8", [128, 128], F32), ("erow", [128, 1], F32)]

W_SPECS = [("g_mix", [1, D]), ("w_in", [1, D, 3072]), ("w_fourier", [1, 512, D]), ("ssm_a_re", [1, 2, 32, 64]),
           ("ssm_a_im", [1, 2, 32, 64]), ("ssm_log_dt", [1, 2, 32]), ("ssm_b_re", [1, 2, 32, 64, 16]),
           ("ssm_b_im", [1, 2, 32, 64, 16]), ("ssm_c_re", [1, 2, 32, 16, 64]), ("ssm_c_im", [1, 2, 32, 16, 64]),
           ("ssm_d", [1, 512]), ("w_glu", [1, 512, 2048]), ("w_out", [1, D, D]), ("g_ffn", [1, D]),
           ("w_router", [1, D, NE]), ("w_exp_gate", [1, NE, D, DEXP]), ("w_exp_up", [1, NE, D, DEXP]),
           ("w_exp_down", [1, NE, DEXP, D]), ("g_ple", [1, D]), ("w_ple_gate", [1, D, D]),
           ("w_ple_proj", [1, 256, D]), ("g_final", [D])]


def build(stage=99, dbg=False):
    nc = bass.Bass("TRN2", target_bir_lowering=False)
    m = MK(nc)
    x = m.dram("x", [S, D], F32, "ExternalInput")
    pin = m.dram("p", [S, 256], F32, "ExternalInput")
    class _LazyW(dict):
        def __missing__(self, n):
            self[n] = m.dram(n, dict(W_SPECS)[n], F32, "ExternalInput")
            return self[n]
    w = _LazyW()
    cst = {n: m.dram(n, shp, dt, "ExternalInput") for n, shp, dt in CONST_SPECS}
    out = m.dram("out", [S, D], F32, "ExternalOutput")
    dbgo = {}

    def dbg_out(name, shape, dt=F32):
        dbgo[name] = m.dram(name, shape, dt, "ExternalOutput")
        return dbgo[name]

    _regs = {}

    def _bc(e):
        if 'bc' not in _regs:
            _regs['bc'] = e.to_reg(NE * CAP - 1)
        return _regs['bc']

    BS = m.dram("BS", [64, 2, 128, 512], BF16)
    YS = m.dram("YS", [S, 512], BF16)
    X1 = m.dram("X1", [S, D], F32)
    H2 = m.dram("H2", [S, D], BF16)
    AFs = m.dram("AFs", [NE, S], F32)
    XG = m.dram("XG", [NE * CAP, D], BF16)
    YE = m.dram("YE", [NE * CAP, D], BF16)

    xC = x.rearrange("(p j) d -> j p d", j=64)
    pC = pin.rearrange("(p j) d -> j p d", j=64)
    outC = out.rearrange("(p j) d -> j p d", j=64)
    X1C = X1.rearrange("(p j) d -> j p d", j=64)
    H2C = H2.rearrange("(p j) d -> j p d", j=64)
    YSC = YS.rearrange("(p j) d -> j p d", j=64)
    xA = x.rearrange("(n1 n2) d -> n2 n1 d", n2=128)

    PW = 7450
    pers = m.sb("pers", [128, PW], F32)
    pc = Carve(pers, PW, "pers")
    ident_bf = pc.bf16(128)
    ident_f = pc.f32(128)
    gbc = {n: pc.f32(D) for n in ("g_mix", "g_ffn", "g_ple", "g_final")}
    AFF_TM = pc.f32(NT, NE)
    VALM = pc.f32(NT, NE)
    POSI = pc.i32(NT, NE)
    eps_t = pc.f32(1)
    ones_t = pc.f32(1)
    AW = 212800 // 4 - PW - 10
    arena = m.sb("arena", [128, AW], F32)
    PB = [m.ps("pb%d" % i, [128, 512], F32) for i in range(8)]

    def pbf(i, *shape):
        v = PB[i][:, :].bitcast(BF16)
        n = int(np.prod(shape))
        return Carve._shape(v[:, 0:n], shape)

    m.ld("sp", ident_bf, cst["ident_bf"], [], ["ident_bf"])
    m.ld("sp", ident_f, cst["ident_f"], [], ["ident_f"])
    for n in gbc:
        src = w[n][0:1, :] if n != "g_final" else w[n].rearrange("(o d) -> o d", o=1)
        m.ld("sp", gbc[n], src.partition_broadcast(128), [], ["gbc_" + n])
    m.memset("dve", eps_t, 1e-6, ["eps"])
    m.memset("dve", ones_t, 1.0, ["ones"])

    def load_cast(dst3, src3, key, nsplit):
        A = dst3.shape[1]
        step = max(1, A // nsplit)
        for a0 in range(0, A, step):
            a1 = min(A, a0 + step)
            m.dma("pool", (lambda d=dst3[:, a0:a1, :], s=src3[:, a0:a1, :]: (lambda e: e.dma_start(out=d, in_=s)))(), [], [key])

    def rmsnorm(xt, kx, g, kg, outt, kout, junk, kjunk, ss, kss, eng2="dve"):
        m.act(junk, xt, ACTF.Square, [kx], [kjunk, kss], accum=ss)
        m.ts("dve", ss, ss, 1.0 / D, eps_t[:, 0:1], ALU.mult, ALU.add, [kss, "eps"], [kss])
        m.act(ss, ss, ACTF.Sqrt, [kss], [kss])
        m.op("dve", lambda e: e.reciprocal(ss, ss), [kss], [kss])
        m.stt(eng2 if eng2 == "dve" else "dve", outt, xt, ss[:, 0:1], g, ALU.mult, ALU.mult, [kx, kss, kg], [kout])

    ca = Carve(arena, AW, "A")
    UT = ca.bf16(4, S)
    mark_after_UT = ca.o
    winA = ca.bf16(8, 1024)
    xt2 = [ca.f32(D) for _ in range(2)]
    t1t = [ca.bf16(2, 128) for _ in range(2)]
    junkA = ca.f32(D)
    hb = ca.bf16(D)
    hT = ca.bf16(8, 128)
    ufb = ca.bf16(512)
    bsb = [ca.bf16(2, 512) for _ in range(2)]
    ssA = [ca.f32(1) for _ in range(2)]

    w_in_v = w["w_in"][0].rearrange("(kc p) n -> p kc n", p=128)
    load_cast(winA, w_in_v[:, :, 0:1024], "winA", 4)

    UTv = [UT[:, c, :].rearrange("p (n1 n2) -> p n2 n1", n2=128) for c in range(4)]
    xt3 = [xt2[0], xt2[1], ca.f32(D)]
    t1t3 = [t1t[0], t1t[1], ca.bf16(2, 128)]
    ssA3 = [ssA[0], ssA[1], ca.f32(1)]
    hb2 = [hb, ca.bf16(D)]
    hT2 = [hT, ca.bf16(8, 128)]

    def loadA(a):
        sl = a % 3
        m.ld("sp", xt3[sl][0:64, :], xA[2 * a], [], ["xA%d" % sl])
        m.ld("sp", xt3[sl][64:128, :], xA[2 * a + 1], [], ["xA%d" % sl])
        m.ld("sp", t1t3[sl], cst["t1"][a], [], ["t1t%d" % sl])

    def A1(a):
        s3 = a % 3
        sl = a % 2
        if a + 1 < NT:
            loadA(a + 1)
        rmsnorm(xt3[s3], "xA%d" % s3, gbc["g_mix"], "gbc_g_mix", hb2[sl], "hb%d" % sl, junkA, "junkA", ssA3[s3], "ssA%d" % s3)
        yield
        hTp = pbf(0, 8, 128)
        for kc in range(8):
            m.tr(hTp[:, kc, :], hb2[sl][:, kc * 128:(kc + 1) * 128], ident_bf, ["hb%d" % sl, "ident_bf"], ["P0"])
        m.cp("act", hT2[sl], hTp, ["P0"], ["hT%d" % sl])
        yield

    def A2(a):
        s3 = a % 3
        sl = a % 2
        hT_ = hT2[sl]
        kh = "hT%d" % sl
        m.mm(PB[1][:, :], [(hT_[:, kc, :], winA[:, kc, 0:512]) for kc in range(8)], [kh, "winA"], ["P1"])
        m.cp("act", ufb, PB[1][:, :], ["P1"], ["ufb"])
        yield
        m.mm(PB[2][:, :], [(t1t3[s3][:, 0, :], ufb)], ["ufb", "t1t%d" % s3], ["P2"])
        m.mm(PB[3][:, :], [(t1t3[s3][:, 1, :], ufb)], ["ufb", "t1t%d" % s3], ["P3"])
        kb = "bsb%d" % sl
        m.cp("dve", bsb[sl][:, 0, :], PB[2][:, :], ["P2"], [kb])
        m.cp("dve", bsb[sl][:, 1, :], PB[3][:, :], ["P3"], [kb])
        for off in range(2):
            m.ld("sp", BS[:, :, 2 * a + off, :], bsb[sl][64 * off:64 * off + 64, :, :], [kb], ["BS"])
        yield
        up = PB[4 + (a % 2)][:, :].rearrange("p (c t) -> p c t", c=4)
        kp = "P%d" % (4 + (a % 2))
        for c in range(4):
            m.mm(up[:, c, :], [(winA[:, kc, 512 + c * 128:512 + (c + 1) * 128], hT_[:, kc, :]) for kc in range(8)],
                 [kh, "winA"], [kp])
        yield
        for c in range(4):
            for off in range(2):
                m.cp("act" if c % 2 else "dve", UTv[c][:, 2 * a + off, :], up[:, c, 64 * off:64 * off + 64], [kp], ["UT"])
        yield

    def _interleave(g1, g2):
        d1 = d2 = False
        while not (d1 and d2):
            if not d1:
                try:
                    next(g1)
                except StopIteration:
                    d1 = True
            if not d2:
                try:
                    next(g2)
                except StopIteration:
                    d2 = True

    loadA(0)
    _interleave(A1(0), iter(()))
    for a in range(NT):
        _interleave(A2(a), A1(a + 1) if a + 1 < NT else iter(()))
    if stage == 1:
        d_ut = dbg_out("d_ut", [4, 128, S], BF16)
        for c in range(4):
            m.ld("sp", d_ut[c], UT[:, c, :], ["UT"], ["dbg"])
        d_bs = dbg_out("d_bs", [64, 2, 128, 512], BF16)
        m.ld("sp", d_bs, BS, ["BS"], ["dbg"])
        return nc, m, dbgo, w

    m.barrier()
    cb_ = Carve(arena, AW, "B")
    cb_.o = mark_after_UT
    LB = 16
    NB_ = S // LB
    ROT = [cb_.f32(NB_ + 1), cb_.f32(NB_ + 1)]
    iota = cb_.f32(TB + 1)
    wk = [cb_.f32(TB) for _ in range(6)]
    wk = [wk[0], wk[1], wk[2], wk[3], wk[0], wk[2], wk[4], wk[5]]
    Xs = [[[cb_.bf16(NB_ + 2) for _ in range(2)] for _ in range(4)] for _ in range(2)]
    TBt = cb_.bf16(LB, 2, 128)
    TCx = [cb_.bf16(4, LB + 1, 2, 32) for _ in range(2)]
    KT = [cb_.bf16(LB, 128) for _ in range(2)]
    K0acc = cb_.f32(128)
    BsP4 = cb_.f32(4, 2, 128)
    BsP4b = cb_.bf16(4, 2, 128)
    Pt = cb_.f32(2, LB + 1)
    tbB = cb_.f32(8, 128)
    tbO = cb_.bf16(LB, 2, 128)
    ardt = cb_.f32(2, 16); thr16 = cb_.f32(2, 16); r16 = cb_.f32(2, 16)
    p17 = [cb_.f32(LB + 1) for _ in range(6)]
    p17i = cb_.i32(LB + 1)
    tcA = cb_.f32(LB + 1, 16)
    tcB = cb_.f32(LB + 1, 16)
    are = cb_.f32(2, 16); aim = cb_.f32(2, 16); ldt = cb_.f32(2, 16)
    dtv = cb_.f32(2, 16); th = cb_.f32(2, 16); thr = cb_.f32(2, 16); r1 = cb_.f32(2, 16)
    cth = cb_.f32(2, 16); sth = cb_.f32(2, 16); cfr = cb_.f32(2, 16); cfi = cb_.f32(2, 16)
    sm = [cb_.f32(2, 16) for _ in range(6)]
    smi = cb_.i32(2, 16)
    sq = [cb_.f32(2, 16) for _ in range(3)]
    sqi = cb_.i32(2, 16)
    Cc = cb_.f32(2, 2, 16, 16)
    Cq = cb_.f32(2, 2, 16, 16)
    tbA = Cc.rearrange("p a b c d -> p (a b c d)").rearrange("p (t n) -> p t n", n=128)
    dvec = cb_.f32(4)
    tki = cb_.i32(TB + 1)
    tkf = cb_.f32(TB + 1)
    targ = cb_.f32(TB + 1)
    tmk = cb_.f32(TB + 1)
    gb = cb_.bf16(4, 128)
    tkf2 = cb_.f32(TB + 1)

    m.ld("sp", iota, cst["iota"], [], ["iota"])
    m.ld("sp", dvec, w["ssm_d"][0].rearrange("(c p) -> p c", p=128), [], ["dvec"], slow=True)
    for d in range(2):
        m.ld("sp", are[:, d, :], w["ssm_a_re"][0, d].rearrange("(sc h) n -> (h n) sc", h=2), [], ["are"], slow=True)
        m.ld("sp", aim[:, d, :], w["ssm_a_im"][0, d].rearrange("(sc h) n -> (h n) sc", h=2), [], ["aim"], slow=True)
        ldv = w["ssm_log_dt"][0, d].rearrange("(sc h) -> h sc", h=2)
        for h in range(2):
            m.ld("sp", ldt[64 * h:64 * h + 64, d, :], ldv[h:h + 1, :].partition_broadcast(64), [], ["ldt"], slow=True)
            for r, nm in enumerate(("ssm_c_re", "ssm_c_im")):
                cv = w[nm][0, d].rearrange("(sc h) o n -> h n sc o", h=2)
                for sc_ in range(16):
                    m.ld("sp", Cc[64 * h:64 * h + 64, d, r, sc_, :], cv[h][:, sc_, :], [], ["Cc"], slow=True)

    def sincos(arg, karg, n, sin_out, ksin, cos_out, kcos, kf, ki, red, msk):
        ks = "sc_scr"
        m.ts("dve", kf, arg, 1.0 / TWO_PI, None, ALU.mult, None, [karg], [ks])
        m.cp("dve", ki, kf, [ks], [ks])
        m.cp("dve", kf, ki, [ks], [ks])
        m.stt("dve", red, kf, -CW1, arg, ALU.mult, ALU.add, [ks, karg], [ks])
        m.stt("dve", red, kf, -CW2, red, ALU.mult, ALU.add, [ks], [ks])
        for shift, o, ko in ((0.0, sin_out, ksin), (math.pi / 2, cos_out, kcos)):
            m.ts("dve", kf, red, shift, None, ALU.add, None, [ks], [ks])
            m.op("dve", lambda e: e.tensor_single_scalar(msk, kf, math.pi, ALU.is_gt), [ks], [ks])
            m.stt("dve", kf, msk, -TWO_PI, kf, ALU.mult, ALU.add, [ks], [ks])
            m.op("dve", lambda e: e.tensor_single_scalar(msk, kf, -math.pi, ALU.is_lt), [ks], [ks])
            m.stt("dve", kf, msk, TWO_PI, kf, ALU.mult, ALU.add, [ks], [ks])
            m.act(o, kf, ACTF.Sin, [ks], [ko])

    fl = lambda t: t.rearrange("p a b -> p (a b)")
    KP = ["are", "aim", "ldt"]
    m.act(fl(dtv), fl(ldt), ACTF.Exp, ["ldt"], ["dtv"])
    m.tt("dve", fl(th), fl(aim), fl(dtv), ALU.mult, ["aim", "dtv"], ["th"])
    m.tt("dve", fl(ardt), fl(are), fl(dtv), ALU.mult, ["are", "dtv"], ["ardt"])
    m.act(fl(r1), fl(ardt), ACTF.Exp, ["ardt"], ["r1"])
    m.act(fl(r16), fl(ardt), ACTF.Exp, ["ardt"], ["r16"], scale=float(LB))
    sincos(fl(th), "th", 32, fl(sth), "sth", fl(cth), "cth", fl(sq[0]), fl(sqi), fl(sq[1]), fl(sq[2]))
    m.ts("dve", fl(sm[1]), fl(th), 1.0 / TWO_PI, None, ALU.mult, None, ["th"], ["sm1"])
    m.cp("dve", fl(smi), fl(sm[1]), ["sm1"], ["smi"])
    m.cp("dve", fl(sm[1]), fl(smi), ["smi"], ["sm1"])
    m.stt("dve", fl(thr), fl(sm[1]), -CW1, fl(th), ALU.mult, ALU.add, ["sm1", "th"], ["thr"])
    m.stt("dve", fl(thr), fl(sm[1]), -CW2, fl(thr), ALU.mult, ALU.add, ["sm1", "thr"], ["thr"])
    m.tt("dve", fl(sm[0]), fl(r1), fl(cth), ALU.mult, ["r1", "cth"], ["sm0"])
    m.ts("dve", fl(sm[0]), fl(sm[0]), -1.0, None, ALU.add, None, ["sm0"], ["sm0"])
    m.tt("dve", fl(sm[1]), fl(r1), fl(sth), ALU.mult, ["r1", "sth"], ["sm1"])
    m.tt("dve", fl(sm[2]), fl(are), fl(are), ALU.mult, ["are"], ["sm2"])
    m.tt("dve", fl(sm[3]), fl(aim), fl(aim), ALU.mult, ["aim"], ["sm3"])
    m.tt("dve", fl(sm[2]), fl(sm[2]), fl(sm[3]), ALU.add, ["sm2", "sm3"], ["sm2"])
    m.op("dve", lambda e: e.reciprocal(fl(sm[2]), fl(sm[2])), ["sm2"], ["sm2"])
    m.tt("dve", fl(sm[3]), fl(sm[0]), fl(are), ALU.mult, ["sm0", "are"], ["sm3"])
    m.tt("dve", fl(sm[4]), fl(sm[1]), fl(aim), ALU.mult, ["sm1", "aim"], ["sm4"])
    m.tt("dve", fl(sm[3]), fl(sm[3]), fl(sm[4]), ALU.add, ["sm3", "sm4"], ["sm3"])
    m.tt("dve", fl(cfr), fl(sm[3]), fl(sm[2]), ALU.mult, ["sm3", "sm2"], ["cfr"])
    m.tt("dve", fl(sm[3]), fl(sm[1]), fl(are), ALU.mult, ["sm1", "are"], ["sm3"])
    m.tt("dve", fl(sm[4]), fl(sm[0]), fl(aim), ALU.mult, ["sm0", "aim"], ["sm4"])
    m.tt("dve", fl(sm[3]), fl(sm[3]), fl(sm[4]), ALU.subtract, ["sm3", "sm4"], ["sm3"])
    m.tt("dve", fl(cfi), fl(sm[3]), fl(sm[2]), ALU.mult, ["sm3", "sm2"], ["cfi"])
    for d in range(2):
        cr = cfr[:, d, :].unsqueeze(2).to_broadcast([128, 16, 16])
        ci = cfi[:, d, :].unsqueeze(2).to_broadcast([128, 16, 16])
        t0 = wk[0][:, 0:256].rearrange("p (a b) -> p a b", b=16)
        t1_ = wk[1][:, 0:256].rearrange("p (a b) -> p a b", b=16)
        m.tt("dve", t0, Cc[:, d, 0], cr, ALU.mult, ["Cc", "cfr"], ["wk0"])
        m.tt("dve", t1_, Cc[:, d, 1], ci, ALU.mult, ["Cc", "cfi"], ["wk1"])
        m.tt("dve", Cq[:, d, 0], t0, t1_, ALU.subtract, ["wk0", "wk1"], ["Cq"])
        m.tt("dve", t0, Cc[:, d, 0], ci, ALU.mult, ["Cc", "cfi"], ["wk0"])
        m.tt("dve", t1_, Cc[:, d, 1], cr, ALU.mult, ["Cc", "cfr"], ["wk1"])
        m.tt("dve", t0, t0, t1_, ALU.add, ["wk0", "wk1"], ["wk0"])
        m.ts("dve", Cq[:, d, 1], t0, -1.0, None, ALU.mult, None, ["wk0"], ["Cq"])

    pk = [cb_.f32(TB) for _ in range(2)]
    gl = [pk[0], pk[1], wk[3]]
    m.ts("dve", fl(sm[1]), fl(thr), float(LB) / TWO_PI, None, ALU.mult, None, ["thr"], ["sm1"])
    m.cp("dve", fl(smi), fl(sm[1]), ["sm1"], ["smi"])
    m.cp("dve", fl(sm[1]), fl(smi), ["smi"], ["sm1"])
    m.ts("dve", fl(thr16), fl(thr), float(LB), None, ALU.mult, None, ["thr"], ["thr16"])
    m.stt("dve", fl(thr16), fl(sm[1]), -CW1, fl(thr16), ALU.mult, ALU.add, ["sm1", "thr16"], ["thr16"])
    m.stt("dve", fl(thr16), fl(sm[1]), -CW2, fl(thr16), ALU.mult, ALU.add, ["sm1", "thr16"], ["thr16"])
    YSj = YS.rearrange("(q c j) d -> j c q d", j=LB, c=128)
    for d in range(2):
        for scl in range(4):
            for r in range(2):
                m.memset("pool", Xs[d][scl][r], 0.0, ["X%d_%d" % (d, scl)])
    for chunk in range(4):
        UTc = UT[:, chunk, :]
        for d in range(2):
            kT = "TCx%d" % d
            m.memset("pool", TCx[d], 0.0, [kT])
            m.memset("pool", BsP4, 0.0, ["BsP4"])
            for scl in range(4):
                sc = chunk * 4 + scl
                for r, nm in enumerate(("ssm_b_re", "ssm_b_im")):
                    bv = w[nm][0, d].rearrange("(sc h) n i -> sc h n i", h=2)
                    for h in range(2):
                        c0_ = scl * 32 + h * 16
                        m.ld("sp", BsP4[64 * h:64 * h + 64, scl, r, c0_:c0_ + 16], bv[sc, h], [], ["BsP4"])
            m.cp("dve", BsP4b, BsP4, ["BsP4"], ["BsP4b"])
            for scl in range(4):
                sc = chunk * 4 + scl
                i17 = iota[:, 0:LB + 1]
                m.ts("dve", p17[0], i17, thr[:, d, sc:sc + 1], None, ALU.mult, None, ["iota", "thr"], ["p17_0"])
                sincos(p17[0], "p17_0", LB + 1, p17[1], "p17_1", p17[2], "p17_2", p17[3], p17i, p17[4], p17[5])
                m.act(p17[3], i17, ACTF.Exp, ["iota", "ardt", "sc_scr"], ["sc_scr"], scale=ardt[:, d, sc:sc + 1])
                m.tt("dve", Pt[:, 0, :], p17[3], p17[2], ALU.mult, ["sc_scr", "p17_2"], ["Pt"])
                m.tt("dve", Pt[:, 1, :], p17[3], p17[1], ALU.mult, ["sc_scr", "p17_1"], ["Pt"])
                for h in range(2):
                    rows = slice(64 * h, 64 * h + 64)
                    q0 = Cq[rows, d, 0, sc, :].unsqueeze(1).to_broadcast([64, LB + 1, 16])
                    q1 = Cq[rows, d, 1, sc, :].unsqueeze(1).to_broadcast([64, LB + 1, 16])
                    pre = Pt[rows, 0, :].unsqueeze(2).to_broadcast([64, LB + 1, 16])
                    pim = Pt[rows, 1, :].unsqueeze(2).to_broadcast([64, LB + 1, 16])
                    m.tt("pool", tcA[rows], q0, pre, ALU.mult, ["Cq", "Pt"], ["tcA"])
                    m.tt("pool", tcB[rows], q1, pim, ALU.mult, ["Cq", "Pt"], ["tcB"])
                    m.tt("pool", TCx[d][rows, scl, :, 0, 16 * h:16 * h + 16], tcA[rows], tcB[rows], ALU.add, ["tcA", "tcB"], [kT])
                    m.tt("pool", tcA[rows], q1, pre, ALU.mult, ["Cq", "Pt"], ["tcA"])
                    m.tt("pool", tcB[rows], q0, pim, ALU.mult, ["Cq", "Pt"], ["tcB"])
                    m.tt("pool", TCx[d][rows, scl, :, 1, 16 * h:16 * h + 16], tcA[rows], tcB[rows], ALU.subtract, ["tcA", "tcB"], [kT])
                m.memset("pool", tbO, 0.0, ["tbO"])
                for h in range(2):
                    rows = slice(64 * h, 64 * h + 64)
                    cols = slice(scl * 32 + 16 * h, scl * 32 + 16 * h + 16)
                    prev = Pt[rows, 0, 0:LB][:, ::-1].unsqueeze(2).to_broadcast([64, LB, 16])
                    pimv = Pt[rows, 1, 0:LB][:, ::-1].unsqueeze(2).to_broadcast([64, LB, 16])
                    bre = BsP4[rows, scl, 0, cols].unsqueeze(1).to_broadcast([64, LB, 16])
                    bim = BsP4[rows, scl, 1, cols].unsqueeze(1).to_broadcast([64, LB, 16])
                    tA = tbB[rows].rearrange("p a b -> p (a b)")[:, 0:256].rearrange("p (a b) -> p a b", b=16)
                    tB_ = tbB[rows].rearrange("p a b -> p (a b)")[:, 256:512].rearrange("p (a b) -> p a b", b=16)
                    m.tt("dve", tA, bre, prev, ALU.mult, ["BsP4", "Pt"], ["tbB"])
                    m.tt("dve", tB_, bim, pimv, ALU.mult, ["BsP4", "Pt"], ["tbB"])
                    m.tt("dve", tbO[rows, :, 0, cols], tA, tB_, ALU.subtract, ["tbB"], ["tbO"])
                    m.tt("dve", tA, bre, pimv, ALU.mult, ["BsP4", "Pt"], ["tbB"])
                    m.tt("dve", tB_, bim, prev, ALU.mult, ["BsP4", "Pt"], ["tbB"])
                    m.tt("dve", tbO[rows, :, 1, cols], tA, tB_, ALU.add, ["tbB"], ["tbO"])
                for g4 in range(4):
                    tpb = pbf(6 + g4 % 2, 4, 2, 128)
                    kb_ = "P%d" % (6 + g4 % 2)
                    for t4 in range(4):
                        for r in range(2):
                            m.tr(tpb[:, t4, r, :], tbO[:, 4 * g4 + t4, r, :], ident_bf, ["tbO", "ident_bf"], [kb_])
                    m.cp("act", TBt[:, 4 * g4:4 * g4 + 4, :, :], tpb, [kb_], ["TBt"])
                for r in range(2):
                    m.mm(PB[r][:, :], [(TBt[:, (t if d == 0 else LB - 1 - t), r, :], UTc[:, t::LB]) for t in range(LB)], ["TBt", "UT"], ["P%d" % r])
                m.ts("dve", targ, iota, thr16[:, d, sc:sc + 1], None, ALU.mult, None, ["iota", "thr16"], ["targ"])
                sincos(targ, "targ", TB + 1, ROT[1], "rot", ROT[0], "rot", tkf, tki, tmk, tkf2)
                pr = PB[0][:, :] if d == 0 else PB[0][:, ::-1]
                pi_ = PB[1][:, :] if d == 0 else PB[1][:, ::-1]
                COSv = ROT[0][:, 0:NB_]
                SINv = ROT[1][:, 0:NB_]
                m.tt("dve", wk[0], pr, COSv, ALU.mult, ["P0", "rot"], ["wk0"])
                m.tt("dve", wk[1], pi_, SINv, ALU.mult, ["P1", "rot"], ["wk1"])
                m.tt("dve", wk[2], pi_, COSv, ALU.mult, ["P1", "rot"], ["wk2"])
                m.tt("dve", wk[3], pr, SINv, ALU.mult, ["P0", "rot"], ["wk3"])
                m.tt("dve", wk[0], wk[0], wk[1], ALU.add, ["wk0", "wk1"], ["wk0"])
                m.tt("dve", wk[2], wk[2], wk[3], ALU.subtract, ["wk2", "wk3"], ["wk2"])
                r16b = r16[:, d, sc:sc + 1].to_broadcast([128, NB_])
                m.op("dve", (lambda o=wk[6], a=r16b, b_=wk[0]: (lambda e: e.tensor_tensor_scan(o, a, b_, 0.0, ALU.mult, ALU.add)))(), ["wk0", "r16"], ["wk6"])
                m.op("dve", (lambda o=wk[7], a=r16b, b_=wk[2]: (lambda e: e.tensor_tensor_scan(o, a, b_, 0.0, ALU.mult, ALU.add)))(), ["wk2", "r16"], ["wk7"])
                kx_ = "X%d_%d" % (d, scl)
                if d == 0:
                    xre = Xs[d][scl][0][:, 1:NB_ + 1]
                    xim = Xs[d][scl][1][:, 1:NB_ + 1]
                else:
                    xre = Xs[d][scl][0][:, 0:NB_][:, ::-1]
                    xim = Xs[d][scl][1][:, 0:NB_][:, ::-1]
                m.tt("pool", pk[0], wk[6], COSv, ALU.mult, ["wk6", "rot"], ["pk0"])
                m.tt("pool", pk[1], wk[7], SINv, ALU.mult, ["wk7", "rot"], ["pk1"])
                m.tt("pool", xre, pk[0], pk[1], ALU.subtract, ["pk0", "pk1"], [kx_])
                m.tt("pool", pk[0], wk[6], SINv, ALU.mult, ["wk6", "rot"], ["pk0"])
                m.tt("pool", pk[1], wk[7], COSv, ALU.mult, ["wk7", "rot"], ["pk1"])
                m.tt("pool", xim, pk[0], pk[1], ALU.add, ["pk0", "pk1"], [kx_])
            for m_ in range(LB):
                bk = 2 + m_ % 2
                kb_ = "P%d" % bk
                first = True
                for scl in range(4):
                    for r in range(2):
                        m.op("pe", (lambda o=PB[bk][:, scl * 32:scl * 32 + 32], l=BsP4b[:, scl, r, :], rr=TCx[d][:, scl, m_, r, :], st=first, sp_=(scl == 3 and r == 1):
                                    (lambda e: e.matmul(o, l, rr, start=st, stop=sp_)))(), ["BsP4b", kT], [kb_])
                        first = False
                if m_ == 0:
                    if d == 0:
                        m.stt("dve", K0acc, ident_f, dvec[:, chunk:chunk + 1], PB[bk][:, 0:128], ALU.mult, ALU.add, ["ident_f", "dvec", kb_], ["K0acc"])
                    else:
                        m.tt("dve", KT[0][:, 0, :], PB[bk][:, 0:128], K0acc, ALU.add, [kb_, "K0acc"], ["KT"])
                else:
                    m.cp("act", KT[d][:, m_, :], PB[bk][:, 0:128], [kb_], ["KT"])
        for j in range(LB):
            bk = 4 + j % 2
            kb_ = "P%d" % bk
            nmm = 0
            tot = 4 * (LB + 16)
            for q in range(4):
                base = 16 * 128 * q
                for mlag in range(-(LB - 1 - j), j + 1):
                    t_ = j - mlag
                    lhs = UTc[:, base + t_: base + t_ + 16 * 127 + 1: 16]
                    if mlag >= 0:
                        rhs = KT[0][:, mlag, :]
                    else:
                        rhs = KT[1][:, -mlag, :]
                    m.op("pe", (lambda o=PB[bk][:, q * 128:(q + 1) * 128], l=lhs, rr=rhs, st=(nmm == 0), sp_=(nmm == tot - 1):
                                (lambda e: e.matmul(o, l, rr, start=st, stop=sp_)))(), ["UT", "KT"], [kb_])
                    nmm += 1
                for d in range(2):
                    idx = (j + 1) if d == 0 else (LB - j)
                    c0_ = q * 128 if d == 0 else q * 128 + 1
                    for scl in range(4):
                        for r in range(2):
                            m.op("pe", (lambda o=PB[bk][:, q * 128 + scl * 32:q * 128 + scl * 32 + 32], l=Xs[d][scl][r][:, c0_:c0_ + 128], rr=TCx[d][:, scl, idx, r, :], st=(nmm == 0), sp_=(nmm == tot - 1):
                                        (lambda e: e.matmul(o, l, rr, start=st, stop=sp_)))(), ["X%d_%d" % (d, scl), "TCx%d" % d], [kb_])
                            nmm += 1
            assert nmm == tot, (nmm, tot)
            m.cp("dve", gl[0], PB[bk][:, :], [kb_], ["pk0"])
            m.tt("pool", gl[1], gl[0], gl[0], ALU.mult, ["pk0"], ["pk1"])
            m.ts("pool", gl[1], gl[1], 0.044715, 1.0, ALU.mult, ALU.add, ["pk1"], ["pk1"])
            m.tt("pool", gl[1], gl[1], gl[0], ALU.mult, ["pk1", "pk0"], ["pk1"])
            m.act(gl[2], gl[1], ACTF.Sigmoid, ["pk1"], ["wk3"], scale=1.5957691216057308)
            m.tt("dve", gb.rearrange("p q c -> p (q c)"), gl[0], gl[2], ALU.mult, ["pk0", "wk3"], ["gb"])
            m.ld("sp", YSj[j][:, :, chunk * 128:(chunk + 1) * 128], gb, ["gb"], ["YS"])
    if stage == 2:
        d_ys = dbg_out("d_ys", [S, 512], BF16)
        m.ld("sp", d_ys, YS, ["YS"], ["dbg"])
        d_ut = dbg_out("d_ut", [4, 128, S], BF16)
        for c in range(4):
            m.ld("sp", d_ut[c], UT[:, c, :], ["UT"], ["dbg"])
        d_sm = dbg_out("d_sm", [6, 128, 32], F32)
        for i_, (t_, k_) in enumerate(((r1, "r1"), (thr, "thr"), (cfr, "cfr"), (cfi, "cfi"), (cth, "cth"), (sth, "sth"))):
            m.ld("sp", d_sm[i_], fl(t_), [k_], ["dbg"])
        return nc, m, dbgo, w
    m.barrier()
    cc = Carve(arena, AW, "C")
    wing = cc.bf16(8, 2048)
    Wc = cc.bf16(4, 1024)
    Ws = cc.bf16(4, 1024)
    wglu = cc.bf16(4, 2048)
    wout = cc.bf16(8, 1024)
    wrt = cc.f32(8, NE)
    r3 = cc.bf16(2, 256)
    cs128 = cc.f32(2, 128)
    AFFT = cc.f32(S)
    wf32 = AFFT[:, 0:4096].rearrange("p (g n) -> p g n", g=4)
    xtC = [cc.f32(D) for _ in range(2)]
    DtC = [cc.bf16(2, 512) for _ in range(2)]
    hbC = cc.bf16(D)
    hTC = cc.bf16(8, 128)
    G = cc.f32(2048)
    AT = cc.bf16(4, 256)
    ytC2 = [cc.bf16(512) for _ in range(2)]
    ysT = cc.bf16(4, 128)
    sgC = cc.f32(D)
    tvC = cc.f32(D)
    mix = cc.f32(D)
    mbC = cc.bf16(D)
    mTC = cc.bf16(8, 128)
    x1t = cc.f32(D)
    xn = cc.f32(D)
    xnT = cc.f32(8, 128)
    h2b = cc.bf16(D)
    smC = [cc.f32(1) for _ in range(6)]
    lgt = cc.f32(NE)
    ext = cc.f32(NE)

    load_cast(wing, w_in_v[:, :, 1024:3072], "wing", 8)
    load_cast(wglu, w["w_glu"][0].rearrange("(c p) n -> p c n", p=128), "wglu", 4)
    load_cast(wout, w["w_out"][0].rearrange("(c p) n -> p c n", p=128), "wout", 8)
    m.ld("sp", wrt, w["w_router"][0].rearrange("(c p) n -> p c n", p=128), [], ["wrt"])
    m.ld("sp", r3, cst["r3"], [], ["r3"])
    m.ld("sp", cs128, cst["cs128"], [], ["cs128"])
    m.ld("sp", wf32, w["w_fourier"][0].rearrange("(g p) n -> p g n", p=128), [], ["AFFT"])
    for g_ in range(4):
        for half in range(2):
            for r, (Wt, kW) in enumerate(((Wc, "Wc"), (Ws, "Ws"))):
                bk = 1 + (2 * half + r) % 2
                m.mm(PB[bk][:, :], [(cs128[:, r, :], wf32[:, g_, half * 512:(half + 1) * 512])], ["cs128", "AFFT"], ["P%d" % bk])
                m.cp("dve", Wt[:, g_, half * 512:(half + 1) * 512], PB[bk][:, :], ["P%d" % bk], [kW])

    def loadC(j):
        sl = j % 2
        m.ld("sp", xtC[sl], xC[j], [], ["xC%d" % sl])
        m.ld("sp", DtC[sl], BS[j].rearrange("r n c -> n r c"), ["BS"], ["DtC%d" % sl])
        m.ld("sp", ytC2[sl], YSC[j], ["YS"], ["ytC%d" % sl])
    x1t2 = [x1t, cc.f32(D)]

    def main_steps(j):
        sl = j % 2
        kx = "xC%d" % sl
        kd = "DtC%d" % sl
        ytC = ytC2[sl]
        kyt = "ytC%d" % sl
        x1t_ = x1t2[sl]
        kx1 = "x1t%d" % sl
        if j + 1 < NT:
            loadC(j + 1)
        rmsnorm(xtC[sl], kx, gbc["g_mix"], "gbc_g_mix", hbC, "hbC", mix, "mix", smC[0], "smC0")
        hTp = pbf(0, 8, 128)
        for kc in range(8):
            m.tr(hTp[:, kc, :], hbC[:, kc * 128:(kc + 1) * 128], ident_bf, ["hbC", "ident_bf"], ["P0"])
        m.cp("act", hTC, hTp, ["P0"], ["hTC"])
        yield
        for q in range(4):
            bk = 1 + q % 2
            m.mm(PB[bk][:, :], [(hTC[:, kc, :], wing[:, kc, q * 512:(q + 1) * 512]) for kc in range(8)], ["hTC", "wing"], ["P%d" % bk])
            m.act(G[:, q * 512:(q + 1) * 512], PB[bk][:, :], ACTF.Sigmoid, ["P%d" % bk], ["G"])
            if q % 2:
                yield
        for c in range(4):
            bk = 3 + c // 2
            o_ = PB[bk][:, (c % 2) * 256:(c % 2) * 256 + 256]
            m.mm(o_, [(DtC[sl][:, 0, c * 128:(c + 1) * 128], r3[:, 0, :]), (DtC[sl][:, 1, c * 128:(c + 1) * 128], r3[:, 1, :])], [kd, "r3"], ["P%d" % bk])
        m.cp("dve", AT[:, 0:2, :], PB[3][:, :].rearrange("p (c n) -> p c n", c=2), ["P3"], ["AT"])
        m.cp("dve", AT[:, 2:4, :], PB[4][:, :].rearrange("p (c n) -> p c n", c=2), ["P4"], ["AT"])
        ytp = pbf(7, 4, 128)
        for c in range(4):
            m.tr(ytp[:, c, :], ytC[:, c * 128:(c + 1) * 128], ident_bf, [kyt, "ident_bf"], ["P7"])
        m.cp("act", ysT, ytp, ["P7"], ["ysT"])
        yield
        for half in range(2):
            bk = 5 + half
            pairs = []
            for c in range(4):
                pairs.append((AT[:, c, 0:128], Wc[:, c, half * 512:(half + 1) * 512]))
                pairs.append((AT[:, c, 128:256], Ws[:, c, half * 512:(half + 1) * 512]))
            m.mm(PB[bk][:, :], pairs, ["AT", "Wc", "Ws"], ["P%d" % bk])
            m.tt("dve", mix[:, half * 512:(half + 1) * 512], PB[bk][:, :], G[:, half * 512:(half + 1) * 512], ALU.mult, ["P%d" % bk, "G"], ["mix"])
        yield
        for half in range(2):
            bg = 5 + half
            bv = 1 + half
            hs = slice(half * 512, (half + 1) * 512)
            m.mm(PB[bg][:, :], [(ysT[:, c, :], wglu[:, c, 1024 + half * 512:1024 + (half + 1) * 512]) for c in range(4)], ["ysT", "wglu"], ["P%d" % bg])
            m.act(sgC[:, hs], PB[bg][:, :], ACTF.Sigmoid, ["P%d" % bg], ["sgC"])
            m.mm(PB[bv][:, :], [(ysT[:, c, :], wglu[:, c, half * 512:(half + 1) * 512]) for c in range(4)], ["ysT", "wglu"], ["P%d" % bv])
            m.tt("dve", tvC[:, hs], PB[bv][:, :], sgC[:, hs], ALU.mult, ["P%d" % bv, "sgC"], ["tvC"])
            m.tt("pool", tvC[:, hs], tvC[:, hs], G[:, 1024 + half * 512:1024 + (half + 1) * 512], ALU.mult, ["tvC", "G"], ["tvC"])
            m.tt("dve", mbC[:, hs], mix[:, hs], tvC[:, hs], ALU.add, ["mix", "tvC"], ["mbC"])
            yield
        mTp = pbf(0, 8, 128)
        for kc in range(8):
            m.tr(mTp[:, kc, :], mbC[:, kc * 128:(kc + 1) * 128], ident_bf, ["mbC", "ident_bf"], ["P0"])
        m.cp("act", mTC, mTp, ["P0"], ["mTC"])
        yield
        for half in range(2):
            bk = 3 + half
            hs = slice(half * 512, (half + 1) * 512)
            m.mm(PB[bk][:, :], [(mTC[:, kc, :], wout[:, kc, hs]) for kc in range(8)], ["mTC", "wout"], ["P%d" % bk])
            m.tt("dve", x1t_[:, hs], PB[bk][:, :], xtC[sl][:, hs], ALU.add, ["P%d" % bk, kx], [kx1])
        m.ld("sp", X1C[j], x1t_, [kx1], ["X1"])
        yield

    def tail_steps(j):
        sl = j % 2
        x1t_ = x1t2[sl]
        kx1 = "x1t%d" % sl
        rmsnorm(x1t_, kx1, gbc["g_ffn"], "gbc_g_ffn", xn, "xn", xn, "xn", smC[1], "smC1")
        m.cp("act", h2b, xn, ["xn"], ["h2b"])
        m.ld("sp", H2C[j], h2b, ["h2b"], ["H2"])
        yield
        for hh in range(2):
            tpv = PB[7][:, :].rearrange("p (c n) -> p c n", c=4)
            for k4 in range(4):
                kc = hh * 4 + k4
                m.tr(tpv[:, k4, :], xn[:, kc * 128:(kc + 1) * 128], ident_f, ["xn", "ident_f"], ["P7"])
            m.cp("act" if hh else "dve", xnT[:, hh * 4:(hh + 1) * 4, :], tpv, ["P7"], ["xnT"])
            yield
        m.mm(PB[7][:, 0:NE], [(xnT[:, kc, :], wrt[:, kc, :]) for kc in range(8)], ["xnT", "wrt"], ["P7"])
        m.cp("dve", lgt, PB[7][:, 0:NE], ["P7"], ["lgt"])
        m.op("dve", lambda e: e.tensor_reduce(smC[2], lgt, mybir.AxisListType.X, ALU.max), ["lgt"], ["smC2"])
        m.ts("dve", smC[3], smC[2], -1.0, None, ALU.mult, None, ["smC2"], ["smC3"])
        yield
        m.act(ext, lgt, ACTF.Exp, ["lgt", "smC3"], ["ext", "smC4"], bias=smC[3][:, 0:1], accum=smC[4])
        m.op("dve", lambda e: e.reciprocal(smC[5], smC[4]), ["smC4"], ["smC5"])
        m.ts("dve", AFF_TM[:, j, :], ext, smC[5][:, 0:1], None, ALU.mult, None, ["ext", "smC5"], ["AFF_TM"])
        yield
        m.tr(PB[7][0:NE, 128:256], AFF_TM[:, j, :], ident_f, ["AFF_TM", "ident_f"], ["P7"])
        m.cp("dve", AFFT[0:NE, j * 128:(j + 1) * 128], PB[7][0:NE, 128:256], ["P7"], ["AFFT"])
        yield

    def interleave(g1, g2):
        d1 = d2 = False
        while not (d1 and d2):
            if not d1:
                try:
                    next(g1)
                except StopIteration:
                    d1 = True
            if not d2:
                try:
                    next(g2)
                except StopIteration:
                    d2 = True

    loadC(0)
    for j in range(NT):
        interleave(main_steps(j), tail_steps(j - 1) if j > 0 else iter(()))
    interleave(tail_steps(NT - 1), iter(()))
    m.ld("sp", AFs, AFFT[0:NE, :], ["AFFT"], ["AFs"])
    if stage == 3:
        d_x1 = dbg_out("d_x1", [S, D], F32)
        m.ld("sp", d_x1, X1, ["X1"], ["dbg"])
        d_aff = dbg_out("d_aff", [NE, S], F32)
        m.ld("sp", d_aff, AFs, ["AFs"], ["dbg"])
        return nc, m, dbgo, w
    m.barrier()
    cd_ = Carve(arena, AW, "D")
    A8 = cd_.f32(1024)
    junkD = cd_.f32(1024)
    Mk = cd_.f32(1024)
    cum = cd_.f32(1024)
    idxf = cd_.f32(1024)
    POSF = cd_.f32(NT, NE)
    blk8 = cd_.f32(128)
    tri8 = cd_.f32(128)
    erow = cd_.f32(1)
    lo = cd_.f32(1); hi = cd_.f32(1); mid = cd_.f32(1); ge = cd_.f32(1); d1 = cd_.f32(1); d2 = cd_.f32(1); c0 = cd_.f32(1)
    cnt2 = cd_.f32(2)
    m.ld("sp", A8, AFs.rearrange("e (s c) -> (e s) c", s=8), ["AFs"], ["A8"])
    m.ld("sp", blk8, cst["blk8"], [], ["blk8"])
    m.ld("sp", tri8, cst["tri8"], [], ["tri8"])
    m.ld("sp", erow, cst["erow"], [], ["erow"])
    m.memset("dve", lo, 0.0, ["lo"])
    m.memset("dve", hi, 1.0, ["hi"])
    m.memset("dve", cnt2, 0.0, ["cnt2"])
    for it in range(36):
        m.tt("dve", mid, lo, hi, ALU.add, ["lo", "hi"], ["mid"])
        m.ts("dve", mid, mid, 0.5, None, ALU.mult, None, ["mid"], ["mid"])
        m.ts("dve", junkD, A8, mid[:, 0:1], None, ALU.is_gt, ALU.add, ["A8", "mid"], ["junkD", "cnt2"], accum=cnt2[:, 0:1])
        m.mm(PB[0][:, 0:2], [(blk8, cnt2)], ["blk8", "cnt2"], ["P0"])
        m.op("dve", lambda e: e.tensor_single_scalar(ge, PB[0][:, 0:1], CAP - 0.5, ALU.is_gt), ["P0"], ["ge"])
        m.tt("dve", d1, mid, lo, ALU.subtract, ["mid", "lo"], ["d1"])
        m.stt("dve", lo, d1, ge[:, 0:1], lo, ALU.mult, ALU.add, ["d1", "ge", "lo"], ["lo"])
        m.tt("dve", d2, hi, mid, ALU.subtract, ["hi", "mid"], ["d2"])
        m.stt("dve", hi, d2, ge[:, 0:1], mid, ALU.mult, ALU.add, ["d2", "ge", "mid"], ["hi"])
    m.ts("dve", Mk, A8, lo[:, 0:1], None, ALU.is_gt, None, ["A8", "lo"], ["Mk"])
    m.op("dve", lambda e: e.tensor_tensor_scan(cum, ones_t[:, 0:1].to_broadcast([128, 1024]), Mk, 0.0, ALU.mult, ALU.add), ["Mk", "ones"], ["cum"])
    m.cp("dve", cnt2[:, 0:1], cum[:, 1023:1024], ["cum"], ["cnt2"])
    m.mm(PB[0][:, 2:4], [(tri8, cnt2)], ["tri8", "cnt2"], ["P0"])
    m.tt("dve", c0, PB[0][:, 2:3], erow, ALU.add, ["P0", "erow"], ["c0"])
    m.stt("dve", idxf, cum, c0[:, 0:1], Mk, ALU.add, ALU.mult, ["cum", "c0", "Mk"], ["idxf"])
    m.ts("dve", idxf, idxf, BIG, None, ALU.add, None, ["idxf"], ["idxf"])
    POSIv = POSI.rearrange("p (s c) e -> p s c e", s=8)
    POSFv = POSF.rearrange("p (s c) e -> p s c e", s=8)
    for cb in range(8):
        bk = 1 + cb % 2
        m.tr(PB[bk][:, 0:128], idxf[:, cb * 128:(cb + 1) * 128], ident_f, ["idxf", "ident_f"], ["P%d" % bk])
        src = PB[bk][:, 0:128].rearrange("p (e s) -> p s e", s=8)
        m.cp("dve", POSIv[:, :, cb, :], src, ["P%d" % bk], ["POSI"])
        m.cp("dve", POSFv[:, :, cb, :], src, ["P%d" % bk], ["POSF"])
    m.stt("dve", VALM.rearrange("p a b -> p (a b)"), POSF.rearrange("p a b -> p (a b)"), 0.5 * BIG, AFF_TM.rearrange("p a b -> p (a b)"),
          ALU.is_lt, ALU.mult, ["POSF", "AFF_TM"], ["VALM"])
    if stage == 4:
        d_pos = dbg_out("d_pos", [128, NT * NE], F32)
        m.ld("sp", d_pos, POSF.rearrange("p a b -> p (a b)"), ["POSF"], ["dbg"])
        d_val = dbg_out("d_val", [128, NT * NE], F32)
        m.ld("sp", d_val, VALM.rearrange("p a b -> p (a b)"), ["VALM"], ["dbg"])
        return nc, m, dbgo, w
    m.barrier()
    ce = Carve(arena, AW, "E")
    h2t = [ce.bf16(D) for _ in range(4)]
    for j in range(NT):
        sl = j % 4
        kh = "h2t%d" % sl
        m.ld("sp", h2t[sl], H2C[j], ["H2"], [kh])
        for e_ in range(NE // 2):
            m.dma("pool", (lambda src=h2t[sl], ix=POSI[:, j, e_:e_ + 1]: (lambda e: e.indirect_dma_start(
                out=XG, out_offset=bass.IndirectOffsetOnAxis(ap=ix, axis=0), in_=src, in_offset=None,
                bounds_check=_bc(e), oob_is_err=False)))(), [kh, "POSI"], ["XGw%d" % (j * NE + e_)])
    m.barrier()
    cf = Carve(arena, AW, "F")
    xgt = [cf.bf16(D) for _ in range(2)]
    xgT = cf.bf16(8, CAP)
    wd = cf.bf16(NFC, D)
    wg = [cf.bf16(8, 256) for _ in range(2)]
    wu = [cf.bf16(8, 256) for _ in range(2)]
    hidT = cf.bf16(NFC, CAP)
    sil = [cf.f32(512) for _ in range(2)]
    yo = [cf.bf16(D) for _ in range(2)]
    nE = NE if stage != 5 else 1
    cnt_i = 0
    stg_g = cf.f32(8, 256)
    stg_u = cf.f32(8, 256)
    stg_d = cf.f32(2, D)
    h2u = [cf.bf16(D) for _ in range(3)]
    for j in range(NT):
        sl = j % 3
        kh = "h2u%d" % sl
        m.ld("pool", h2u[sl], H2C[j], ["H2"], [kh])
        for e_ in range(NE // 2, NE):
            m.dma("pool", (lambda src=h2u[sl], ix=POSI[:, j, e_:e_ + 1]: (lambda e: e.indirect_dma_start(
                out=XG, out_offset=bass.IndirectOffsetOnAxis(ap=ix, axis=0), in_=src, in_offset=None,
                bounds_check=_bc(e), oob_is_err=False)))(), [kh, "POSI"], ["XGw%d" % (j * NE + e_)])
    xgT2 = [xgT, cf.bf16(8, CAP)]

    def xg_prep(ee):
        dstT = xgT2[ee % 2]
        for ct in range(8):
            sl = ct % 2
            kx = "xgt%d" % sl
            m.ld("sp", xgt[sl], XG[ee * CAP + ct * 128:ee * CAP + (ct + 1) * 128, :], ["XGw"], [kx])
            tp = pbf(0, 8, 128)
            for kc in range(8):
                m.tr(tp[:, kc, :], xgt[sl][:, kc * 128:(kc + 1) * 128], ident_bf, [kx, "ident_bf"], ["P0"])
            m.cp("act" if ct % 2 else "dve", dstT[:, :, ct * 128:(ct + 1) * 128], tp, ["P0"], ["xgT%d" % (ee % 2)])
    for e_ in range(nE):
        wdv = w["w_exp_down"][0, e_].rearrange("(fc p) d -> p fc d", p=128)
        if e_ == NE // 2 and nE > NE // 2:
            m.barrier()
        if e_ == 0 or e_ == NE // 2:
            xg_prep(e_)
        xgT = xgT2[e_ % 2]
        kxT = "xgT%d" % (e_ % 2)
        wgv = w["w_exp_gate"][0, e_].rearrange("(kc p) f -> p kc f", p=128)
        wuv = w["w_exp_up"][0, e_].rearrange("(kc p) f -> p kc f", p=128)
        for pc_ in range(NFC // 2):
            sl = pc_ % 2
            fs = slice(pc_ * 256, (pc_ + 1) * 256)
            m.ld("sp", stg_g, wgv[:, :, fs], [], ["stg_g"])
            m.cp("dve", wg[sl], stg_g, ["stg_g"], ["wg%d" % sl])
            m.ld("sp", stg_u, wuv[:, :, fs], [], ["stg_u"])
            m.cp("act", wu[sl], stg_u, ["stg_u"], ["wu%d" % sl])
            m.ld("sp", stg_d, wdv[:, 2 * pc_:2 * pc_ + 2, :], [], ["stg_d"])
            m.cp("dve", wd[:, 2 * pc_:2 * pc_ + 2, :], stg_d, ["stg_d"], ["wd"])
            for fcl in range(2):
                fc = 2 * pc_ + fcl
                for half in range(2):
                    bg = 1 + cnt_i % 2
                    bu = 3 + cnt_i % 2
                    ss_ = cnt_i % 2
                    cnt_i += 1
                    hs = slice(half * 512, (half + 1) * 512)
                    m.mm(PB[bg][:, :], [(wg[sl][:, kc, fcl * 128:(fcl + 1) * 128], xgT[:, kc, hs]) for kc in range(8)], ["wg%d" % sl, kxT], ["P%d" % bg])
                    m.mm(PB[bu][:, :], [(wu[sl][:, kc, fcl * 128:(fcl + 1) * 128], xgT[:, kc, hs]) for kc in range(8)], ["wu%d" % sl, kxT], ["P%d" % bu])
                    m.act(sil[ss_], PB[bg][:, :], ACTF.Silu, ["P%d" % bg], ["sil%d" % ss_])
                    m.tt("dve", hidT[:, fc, hs], PB[bu][:, :], sil[ss_], ALU.mult, ["P%d" % bu, "sil%d" % ss_], ["hidT"])
        if e_ + 1 < nE and e_ + 1 != NE // 2:
            xg_prep(e_ + 1)
        for ct in range(8):
            sl = ct % 2
            for dh in range(2):
                bk = 5 + dh
                hs = slice(dh * 512, (dh + 1) * 512)
                m.mm(PB[bk][:, :], [(hidT[:, fc, ct * 128:(ct + 1) * 128], wd[:, fc, hs]) for fc in range(NFC)], ["hidT", "wd"], ["P%d" % bk])
                m.cp("act" if dh else "dve", yo[sl][:, hs], PB[bk][:, :], ["P%d" % bk], ["yo%d" % sl])
            m.ld("act", YE[e_ * CAP + ct * 128:e_ * CAP + (ct + 1) * 128, :], yo[sl], ["yo%d" % sl], ["YEw%d" % (e_ * 8 + ct)])
    if stage == 5:
        d_ye = dbg_out("d_ye", [CAP, D], BF16)
        m.ld("sp", d_ye, YE[0:CAP, :], ["YEw"], ["dbg"])
        d_xg = dbg_out("d_xg", [CAP, D], BF16)
        m.ld("sp", d_xg, XG[0:CAP, :], ["XGw"], ["dbg"])
        d_pos = dbg_out("d_pos", [128, NT * NE], F32)
        m.ld("sp", d_pos, POSF.rearrange("p a b -> p (a b)"), [], ["dbg"])
        return nc, m, dbgo, w
    m.barrier()
    cg = Carve(arena, AW, "G")
    wpg = cg.bf16(8, D)
    wpp = cg.bf16(2, D)
    x1g = [cg.f32(D) for _ in range(2)]
    NYB = 8
    ybuf = [cg.bf16(D) for _ in range(NYB)]
    dg = [cg.bf16(128) for _ in range(4)]
    ptt = [cg.f32(256) for _ in range(2)]
    ptb = cg.bf16(256)
    pTg = cg.bf16(2, 128)
    hbg = cg.bf16(D)
    hTg = cg.bf16(8, 128)
    sgg = cg.f32(D)
    jg = cg.f32(D)
    x3 = cg.f32(D)
    og = [cg.f32(D) for _ in range(2)]
    smg = [cg.f32(1) for _ in range(4)]
    load_cast(wpg, w["w_ple_gate"][0].rearrange("(c p) n -> p c n", p=128), "wpg", 8)
    load_cast(wpp, w["w_ple_proj"][0].rearrange("(c p) n -> p c n", p=128), "wpp", 2)
    for b_ in range(NYB):
        m.memset("dve", ybuf[b_], 0.0, ["ybuf%d" % b_])
    gi = 0
    x1g = [x1g[0], x1g[1], cg.f32(D)]
    ptt = [ptt[0], ptt[1], cg.f32(256)]
    gstate = {"gi": 0}

    def loadG(j):
        sl = j % 3
        m.ld("sp", x1g[sl], X1C[j], ["X1"], ["x1g%d" % sl])
        m.ld("sp", ptt[sl], pC[j], [], ["ptt%d" % sl])

    def part1(j):
        sl = j % 3
        kx = "x1g%d" % sl
        accb = (5, 6) if j % 2 == 0 else (2, 4)
        if j + 1 < NT:
            loadG(j + 1)
        for e_ in range(NE):
            gi = gstate["gi"]
            b_ = gi % NYB
            dsl = gi % 4
            gstate["gi"] += 1
            m.dma("pool", (lambda dst=ybuf[b_], ix=POSI[:, j, e_:e_ + 1]: (lambda e: e.indirect_dma_start(
                out=dst, out_offset=None, in_=YE, in_offset=bass.IndirectOffsetOnAxis(ap=ix, axis=0),
                bounds_check=_bc(e), oob_is_err=False)))(), ["YEw", "POSI"], ["ybuf%d" % b_])
            m.ts("dve", dg[dsl], ident_bf, VALM[:, j, e_:e_ + 1], None, ALU.mult, None, ["ident_bf", "VALM"], ["dg%d" % dsl])
            for half in range(2):
                hs = slice(half * 512, (half + 1) * 512)
                m.op("pe", (lambda o=PB[accb[half]][:, :], l=dg[dsl], r=ybuf[b_][:, hs], st=(e_ == 0), sp_=(e_ == NE - 1):
                            (lambda e: e.matmul(o, l, r, start=st, stop=sp_)))(), ["dg%d" % dsl, "ybuf%d" % b_], ["P%d" % accb[half]])
            if e_ % 2:
                yield
        for half in range(2):
            hs = slice(half * 512, (half + 1) * 512)
            m.tt("dve", x1g[sl][:, hs], PB[accb[half]][:, :], x1g[sl][:, hs], ALU.add, ["P%d" % accb[half], kx], [kx])
        yield

    def part2(j):
        sl = j % 3
        kx = "x1g%d" % sl
        m.cp("act", ptb, ptt[sl], ["ptt%d" % sl], ["ptb"])
        ptp = pbf(0, 2, 128)
        for c in range(2):
            m.tr(ptp[:, c, :], ptb[:, c * 128:(c + 1) * 128], ident_bf, ["ptb", "ident_bf"], ["P0"])
        m.cp("act", pTg, ptp, ["P0"], ["pTg"])
        yield
        rmsnorm(x1g[sl], kx, gbc["g_ple"], "gbc_g_ple", hbg, "hbg", jg, "jg", smg[0], "smg0")
        yield
        hp = pbf(7, 8, 128)
        for kc in range(8):
            m.tr(hp[:, kc, :], hbg[:, kc * 128:(kc + 1) * 128], ident_bf, ["hbg", "ident_bf"], ["P7"])
        m.cp("act", hTg, hp, ["P7"], ["hTg"])
        yield
        for half in range(2):
            hs = slice(half * 512, (half + 1) * 512)
            m.mm(PB[1][:, :], [(pTg[:, c, :], wpp[:, c, hs]) for c in range(2)], ["pTg", "wpp"], ["P1"])
            m.mm(PB[3][:, :], [(hTg[:, kc, :], wpg[:, kc, hs]) for kc in range(8)], ["hTg", "wpg"], ["P3"])
            m.act(sgg[:, hs], PB[3][:, :], ACTF.Sigmoid, ["P3"], ["sgg"])
            m.tt("dve", sgg[:, hs], PB[1][:, :], sgg[:, hs], ALU.mult, ["P1", "sgg"], ["sgg"])
            m.tt("dve", x3[:, hs], sgg[:, hs], x1g[sl][:, hs], ALU.add, ["sgg", kx], ["x3"])
            yield
        osl = j % 2
        rmsnorm(x3, "x3", gbc["g_final"], "gbc_g_final", og[osl], "og%d" % osl, jg, "jg", smg[1], "smg1")
        m.ld("sp", outC[j], og[osl], ["og%d" % osl], ["out"])
        yield

    def interleave2(g1, g2):
        d1 = d2 = False
        while not (d1 and d2):
            if not d1:
                try:
                    next(g1)
                except StopIteration:
                    d1 = True
            if not d2:
                try:
                    next(g2)
                except StopIteration:
                    d2 = True

    loadG(0)
    interleave2(part1(0), iter(()))
    for j in range(NT):
        interleave2(part2(j), part1(j + 1) if j + 1 < NT else iter(()))
    return nc, m, dbgo, w


def kernel(**inputs):
    nb = inputs["x"].shape[0]
    nc, m, _, wused = build(stage=99)
    m.finalize(final_wait_keys=["out"])
    cm = host_consts()
    in_maps = []
    for b in range(nb):
        im = {"x": np.ascontiguousarray(inputs["x"][b]), "p": np.ascontiguousarray(inputs["p"][0, b])}
        for n in wused:
            im[n] = np.ascontiguousarray(inputs[n])
        im.update(cm)
        in_maps.append(im)
    res = run_bass_kernel_spmd(nc, in_maps, core_ids=list(range(nb)))
    return np.stack([r["out"] for r in res.results], axis=0).astype(np.float32)
```
